# Optimizing a Trainium2 kernel written in Bass

```python
import jax
import jax.numpy as jnp
from jax import lax
import numpy as np

D_MODEL = 1024
BATCH = 1
SEQ = 16384
DEPTH = 2

GRID_W = 64
CTX_LEN = 256
N_MIXERS = 2
N_MOD = 6
EPS = 1e-6

D_RNN = 1280
RG_BLOCKS = 10
RG_BLOCK_W = D_RNN // RG_BLOCKS
CONV_W = 4
CONV_PAD_L = 2
CONV_PAD_R = CONV_W - 1 - CONV_PAD_L
RG_C = 8.0
RG_A_MIN = 0.9
RG_A_MAX = 0.999

ML_HEADS = 8
ML_DQK = D_MODEL // (2 * ML_HEADS)
ML_DV = D_MODEL // ML_HEADS
ML_CHUNK = 64
ML_QK_W = ML_HEADS * ML_DQK
ML_V_W = ML_HEADS * ML_DV
ML_IN_W = 2 * ML_QK_W + ML_V_W + D_MODEL + 4 * ML_HEADS
ML_FGATE_BIAS_LO = 3.0
ML_FGATE_BIAS_HI = 6.0

N_GROUPS = 4
EXPERTS_PER_GROUP = 8
N_EXPERTS = N_GROUPS * EXPERTS_PER_GROUP
TOP_K = 2
D_EXPERT = 512
MOE_BLOCK = 128

kernel_name = 'bidir_rglru_mlstm_hier_moe_prefix_dit'


def rms_norm(x, g):
    xf = x.astype(jnp.float32)
    y = xf * lax.rsqrt(jnp.mean(xf * xf, axis=-1, keepdims=True) + EPS)
    return (y * g.astype(jnp.float32)).astype(x.dtype)


def modulate(h, shift, scale):
    return h * (1.0 + scale) + shift


def adaln(cond, w, b):
    m = jax.nn.silu(cond) @ w + b
    return jnp.split(m, N_MOD, axis=-1)


def to_colmajor(t, rows):
    b, n, d = t.shape
    return t.reshape(b, rows, GRID_W, d).transpose(0, 2, 1, 3).reshape(b, n, d)


def from_colmajor(t, rows):
    b, n, d = t.shape
    return t.reshape(b, GRID_W, rows, d).transpose(0, 2, 1, 3).reshape(b, n, d)


def centred_dwconv(u, w, bias):
    n = u.shape[1]
    up = jnp.pad(u, ((0, 0), (CONV_PAD_L, CONV_PAD_R), (0, 0)))
    out = bias + up[:, 0:n] * w[0]
    for j in range(1, CONV_W):
        out = out + up[:, j:j + n] * w[j]
    return out


def rglru_coeffs(u, w_a, b_a, w_x, b_x, lam):
    b, n, _ = u.shape
    uf = u.astype(jnp.float32)
    ub = uf.reshape(b, n, RG_BLOCKS, RG_BLOCK_W)
    r = jax.nn.sigmoid(jnp.einsum('bnhi,hij->bnhj', ub, w_a.astype(jnp.float32)).reshape(b, n, D_RNN) + b_a.astype(jnp.float32))
    ig = jax.nn.sigmoid(jnp.einsum('bnhi,hij->bnhj', ub, w_x.astype(jnp.float32)).reshape(b, n, D_RNN) + b_x.astype(jnp.float32))
    log_a = -RG_C * r * jax.nn.softplus(-lam.astype(jnp.float32))
    a = jnp.exp(log_a)
    return a, jnp.sqrt(-jnp.expm1(2.0 * log_a)) * ig * uf


def linear_scan(a, b, h0, reverse):
    def combine(left, right):
        a_l, b_l = left
        a_r, b_r = right
        return a_l * a_r, a_r * b_l + b_r
    a_cum, b_cum = lax.associative_scan(combine, (a, b), reverse=reverse, axis=1)
    return b_cum + a_cum * h0[:, None, :]


def rglru_mixer(h, hc, w_in, conv_w, conv_b, w_a, b_a, w_x, b_x, lam, w_out, ctx_out):
    px = h @ w_in
    pc = hc @ w_in
    ux = centred_dwconv(px[..., D_RNN:], conv_w, conv_b)
    uc = centred_dwconv(pc[..., D_RNN:], conv_w, conv_b)
    zero = jnp.zeros((h.shape[0], D_RNN), jnp.float32)
    hx_dirs = []
    hc_dirs = []
    for d in range(2):
        rev = d == 1
        a_c, b_c = rglru_coeffs(uc, w_a[d], b_a[d], w_x[d], b_x[d], lam[d])
        hcd = linear_scan(a_c, b_c, zero, rev)
        h0 = hcd[:, 0] if rev else hcd[:, -1]
        a_l, b_l = rglru_coeffs(ux, w_a[d], b_a[d], w_x[d], b_x[d], lam[d])
        hx_dirs.append(linear_scan(a_l, b_l, h0, rev))
        hc_dirs.append(hcd)
    y_x = (jax.nn.gelu(px[..., :D_RNN]) * (hx_dirs[0] + hx_dirs[1]).astype(h.dtype)) @ w_out
    y_c = None
    if ctx_out:
        y_c = (jax.nn.gelu(pc[..., :D_RNN]) * (hc_dirs[0] + hc_dirs[1]).astype(h.dtype)) @ w_out
    return y_x, y_c


def mlstm_chunkwise(q, k, v, log_i, log_f, state):
    b, nh, n, _ = q.shape
    dv = v.shape[-1]
    nc = n // ML_CHUNK

    def chunks(t):
        return jnp.moveaxis(t.reshape((b, nh, nc, ML_CHUNK) + t.shape[3:]), 2, 0)

    lower = jnp.tril(jnp.ones((ML_CHUNK, ML_CHUNK), dtype=bool))

    def step(carry, inp):
        c_mat, n_vec, m = carry
        qc, kc, vc, li, lf = inp
        cum_f = jnp.cumsum(lf, axis=-1)
        log_w = jnp.where(lower, cum_f[..., :, None] - cum_f[..., None, :] + li[..., None, :], -jnp.inf)
        log_inter = cum_f + m[..., None]
        m_t = jnp.maximum(log_inter, jnp.max(log_w, axis=-1))
        w_intra = jnp.exp(log_w - m_t[..., None])
        w_inter = jnp.exp(log_inter - m_t)
        s = jnp.einsum('bhtd,bhsd->bhts', qc, kc) * w_intra
        num = w_inter[..., None] * jnp.einsum('bhtd,bhde->bhte', qc, c_mat) + jnp.einsum('bhts,bhse->bhte', s, vc)
        den = w_inter * jnp.einsum('bhtd,bhd->bht', qc, n_vec) + jnp.sum(s, axis=-1)
        h_out = num / jnp.maximum(jnp.abs(den), jnp.exp(-m_t))[..., None]
        m_new = m_t[..., -1]
        w_state = jnp.exp(cum_f[..., -1:] - cum_f + li - m_new[..., None])
        decay = jnp.exp(cum_f[..., -1] + m - m_new)
        kw = kc * w_state[..., None]
        c_new = decay[..., None, None] * c_mat + jnp.einsum('bhsd,bhse->bhde', kw, vc)
        n_new = decay[..., None] * n_vec + jnp.sum(kw, axis=2)
        return (c_new, n_new, m_new), h_out

    final, hs = lax.scan(step, state, (chunks(q), chunks(k), chunks(v), chunks(log_i), chunks(log_f)))
    return jnp.moveaxis(hs, 0, 2).reshape(b, nh, n, dv), final


def mlstm_mixer(h, hc, w_in, b_gates, norm_g, w_out, ctx_out):
    rows = h.shape[1] // GRID_W

    def project(t):
        p = (t @ w_in).astype(jnp.float32)
        b, n, _ = p.shape

        def heads(cols, dh):
            return cols.reshape(b, n, ML_HEADS, dh).transpose(0, 2, 1, 3)

        q = heads(p[..., :ML_QK_W], ML_DQK) * (ML_DQK ** -0.5)
        k = heads(p[..., ML_QK_W:2 * ML_QK_W], ML_DQK)
        v = heads(p[..., 2 * ML_QK_W:2 * ML_QK_W + ML_V_W], ML_DV)
        o = p[..., 2 * ML_QK_W + ML_V_W:2 * ML_QK_W + ML_V_W + D_MODEL]
        g = (p[..., ML_IN_W - 4 * ML_HEADS:] + b_gates.astype(jnp.float32)).reshape(b, n, 4, ML_HEADS).transpose(2, 0, 3, 1)
        return q, k, v, o, g

    qx, kx, vx, ox, gx = project(to_colmajor(h, rows))
    qc, kc, vc, oc, gc = project(hc)
    b = h.shape[0]
    zero = (jnp.zeros((b, ML_HEADS, ML_DQK, ML_DV), jnp.float32),
            jnp.zeros((b, ML_HEADS, ML_DQK), jnp.float32),
            jnp.zeros((b, ML_HEADS), jnp.float32))

    def flip(t):
        return jnp.flip(t, axis=2)

    hc_f, st_f = mlstm_chunkwise(qc, kc, vc, gc[0], jax.nn.log_sigmoid(gc[1]), zero)
    hx_f, _ = mlstm_chunkwise(qx, kx, vx, gx[0], jax.nn.log_sigmoid(gx[1]), st_f)
    hc_b, st_b = mlstm_chunkwise(flip(qc), flip(kc), flip(vc), flip(gc[2]), flip(jax.nn.log_sigmoid(gc[3])), zero)
    hx_b, _ = mlstm_chunkwise(flip(qx), flip(kx), flip(vx), flip(gx[2]), flip(jax.nn.log_sigmoid(gx[3])), st_b)

    def finish(h_f, h_b, o):
        hs = (h_f + h_b).transpose(0, 2, 1, 3)
        hn = hs * lax.rsqrt(jnp.mean(hs * hs, axis=-1, keepdims=True) + EPS) * norm_g.astype(jnp.float32).reshape(ML_HEADS, ML_DV)
        bb, n = hn.shape[:2]
        return (hn.reshape(bb, n, ML_V_W) * jax.nn.sigmoid(o)).astype(h.dtype) @ w_out

    y_x = from_colmajor(finish(hx_f, flip(hx_b), ox), rows)
    y_c = finish(hc_f, flip(hc_b), oc) if ctx_out else None
    return y_x, y_c


def hier_moe(h, w_grp, b_grp, w_exp, b_exp, w_gate, w_up, w_down):
    t_count, d = h.shape
    hf = h.astype(jnp.float32)
    grp_logits = hf @ w_grp.astype(jnp.float32) + b_grp.astype(jnp.float32)
    p_grp = jax.nn.softmax(grp_logits, axis=-1)
    _, grp = lax.top_k(grp_logits, 1)
    p_sel = jnp.take_along_axis(p_grp, grp, axis=-1)
    exp_logits = (hf @ w_exp.astype(jnp.float32) + b_exp.astype(jnp.float32)).reshape(t_count, N_GROUPS, EXPERTS_PER_GROUP)
    in_grp = jnp.take_along_axis(exp_logits, grp[:, :, None], axis=1)[:, 0]
    top_v, top_i = lax.top_k(in_grp, TOP_K)
    wts = jax.nn.softmax(top_v, axis=-1) * p_sel
    eid = grp * EXPERTS_PER_GROUP + top_i

    n_assign = t_count * TOP_K
    e_flat = eid.reshape(-1)
    order = jnp.argsort(e_flat)
    e_s = e_flat[order]
    tok_s = order // TOP_K
    w_s = wts.reshape(-1)[order]
    counts = jnp.bincount(e_flat, length=N_EXPERTS)
    padded = (counts + MOE_BLOCK - 1) // MOE_BLOCK * MOE_BLOCK
    pad_end = jnp.cumsum(padded)
    pad_start = pad_end - padded
    raw_start = jnp.cumsum(counts) - counts
    dest = pad_start[e_s] + jnp.arange(n_assign) - raw_start[e_s]
    n_rows = n_assign + N_EXPERTS * MOE_BLOCK
    n_blocks = n_rows // MOE_BLOCK
    buf = jnp.zeros((n_rows, d), h.dtype).at[dest].set(h[tok_s])
    blk_e = jnp.clip(jnp.searchsorted(pad_end, jnp.arange(n_blocks) * MOE_BLOCK, side='right'), 0, N_EXPERTS - 1)

    def expert_block(args):
        xb, e = args
        return (jax.nn.silu(xb @ w_gate[e]) * (xb @ w_up[e])) @ w_down[e]

    y_buf = lax.map(expert_block, (buf.reshape(n_blocks, MOE_BLOCK, d), blk_e))
    y_s = y_buf.reshape(n_rows, d)[dest]
    out = jax.ops.segment_sum(y_s.astype(jnp.float32) * w_s[:, None], tok_s, num_segments=t_count)
    return out.astype(h.dtype)


def setup_inputs(seed: int = 0) -> dict:
    key = jax.random.key(seed)
    keys = iter(jax.random.split(key, 96))

    def nrm(shape, scale):
        return jax.random.normal(next(keys), shape, jnp.float32) * scale

    d = D_MODEL
    inputs = {
        'x': nrm((BATCH, SEQ, d), 1.0),
        'c': nrm((BATCH, d), 1.0),
        'ctx': nrm((BATCH, CTX_LEN, d), 1.0),
        'c_ctx': nrm((d,), 1.0),
    }
    for i in range(DEPTH):
        p = 'l%d_' % i
        inputs[p + 'ada_w'] = nrm((d, N_MOD * d), 0.5 * d ** -0.5)
        inputs[p + 'ada_b'] = nrm((N_MOD * d,), 0.02)
        inputs[p + 'norm1_g'] = 1.0 + nrm((d,), 0.02)
        inputs[p + 'norm2_g'] = 1.0 + nrm((d,), 0.02)
        if i % N_MIXERS == 0:
            inputs[p + 'rg_w_in'] = nrm((d, 2 * D_RNN), d ** -0.5)
            inputs[p + 'rg_conv_w'] = nrm((CONV_W, D_RNN), CONV_W ** -0.5)
            inputs[p + 'rg_conv_b'] = nrm((D_RNN,), 0.02)
            inputs[p + 'rg_w_a'] = nrm((2, RG_BLOCKS, RG_BLOCK_W, RG_BLOCK_W), RG_BLOCK_W ** -0.5)
            inputs[p + 'rg_b_a'] = nrm((2, D_RNN), 0.02)
            inputs[p + 'rg_w_x'] = nrm((2, RG_BLOCKS, RG_BLOCK_W, RG_BLOCK_W), RG_BLOCK_W ** -0.5)
            inputs[p + 'rg_b_x'] = nrm((2, D_RNN), 0.02)
            a0 = jax.random.uniform(next(keys), (2, D_RNN), jnp.float32, RG_A_MIN ** (1.0 / RG_C), RG_A_MAX ** (1.0 / RG_C))
            inputs[p + 'rg_lambda'] = jnp.log(a0) - jnp.log1p(-a0)
            inputs[p + 'rg_w_out'] = nrm((D_RNN, d), D_RNN ** -0.5)
        else:
            inputs[p + 'ml_w_in'] = nrm((d, ML_IN_W), d ** -0.5)
            fb = jnp.linspace(ML_FGATE_BIAS_LO, ML_FGATE_BIAS_HI, ML_HEADS, dtype=jnp.float32)
            inputs[p + 'ml_b_gates'] = jnp.concatenate([nrm((ML_HEADS,), 0.1), fb + nrm((ML_HEADS,), 0.1),
                                                        nrm((ML_HEADS,), 0.1), fb + nrm((ML_HEADS,), 0.1)])
            inputs[p + 'ml_norm_g'] = 1.0 + nrm((ML_V_W,), 0.02)
            inputs[p + 'ml_w_out'] = nrm((ML_V_W, d), ML_V_W ** -0.5)
        inputs[p + 'moe_w_grp'] = nrm((d, N_GROUPS), d ** -0.5)
        inputs[p + 'moe_b_grp'] = nrm((N_GROUPS,), 0.01)
        inputs[p + 'moe_w_exp'] = nrm((d, N_EXPERTS), d ** -0.5)
        inputs[p + 'moe_b_exp'] = nrm((N_EXPERTS,), 0.01)
        inputs[p + 'moe_w_gate'] = nrm((N_EXPERTS, d, D_EXPERT), d ** -0.5)
        inputs[p + 'moe_w_up'] = nrm((N_EXPERTS, d, D_EXPERT), d ** -0.5)
        inputs[p + 'moe_w_down'] = nrm((N_EXPERTS, D_EXPERT, d), D_EXPERT ** -0.5)
    inputs['final_norm_g'] = 1.0 + nrm((d,), 0.02)
    return inputs


def reference(x, c, ctx, c_ctx,
              l0_ada_w, l0_ada_b, l0_norm1_g, l0_norm2_g,
              l0_rg_w_in, l0_rg_conv_w, l0_rg_conv_b, l0_rg_w_a, l0_rg_b_a, l0_rg_w_x, l0_rg_b_x, l0_rg_lambda, l0_rg_w_out,
              l0_moe_w_grp, l0_moe_b_grp, l0_moe_w_exp, l0_moe_b_exp, l0_moe_w_gate, l0_moe_w_up, l0_moe_w_down,
              l1_ada_w, l1_ada_b, l1_norm1_g, l1_norm2_g,
              l1_ml_w_in, l1_ml_b_gates, l1_ml_norm_g, l1_ml_w_out,
              l1_moe_w_grp, l1_moe_b_grp, l1_moe_w_exp, l1_moe_b_exp, l1_moe_w_gate, l1_moe_w_up, l1_moe_w_down,
              final_norm_g):
    layers = (
        (l0_ada_w, l0_ada_b, l0_norm1_g, l0_norm2_g,
         (l0_rg_w_in, l0_rg_conv_w, l0_rg_conv_b, l0_rg_w_a, l0_rg_b_a, l0_rg_w_x, l0_rg_b_x, l0_rg_lambda, l0_rg_w_out),
         (l0_moe_w_grp, l0_moe_b_grp, l0_moe_w_exp, l0_moe_b_exp, l0_moe_w_gate, l0_moe_w_up, l0_moe_w_down)),
        (l1_ada_w, l1_ada_b, l1_norm1_g, l1_norm2_g,
         (l1_ml_w_in, l1_ml_b_gates, l1_ml_norm_g, l1_ml_w_out),
         (l1_moe_w_grp, l1_moe_b_grp, l1_moe_w_exp, l1_moe_b_exp, l1_moe_w_gate, l1_moe_w_up, l1_moe_w_down)),
    )
    b, n, d = x.shape
    for i in range(DEPTH):
        ada_w, ada_b, g1, g2, mix_p, moe_p = layers[i]
        need_ctx = i < DEPTH - 1
        sh1, sc1, gt1, sh2, sc2, gt2 = [m[:, None, :] for m in adaln(c, ada_w, ada_b)]
        csh1, csc1, cgt1, csh2, csc2, cgt2 = adaln(c_ctx, ada_w, ada_b)
        hx = modulate(rms_norm(x, g1), sh1, sc1)
        hc = modulate(rms_norm(ctx, g1), csh1, csc1)
        if i % N_MIXERS == 0:
            yx, yc = rglru_mixer(hx, hc, *mix_p, ctx_out=need_ctx)
        else:
            yx, yc = mlstm_mixer(hx, hc, *mix_p, ctx_out=need_ctx)
        x = x + gt1 * yx
        h2 = modulate(rms_norm(x, g2), sh2, sc2)
        if need_ctx:
            ctx = ctx + cgt1 * yc
            hc2 = modulate(rms_norm(ctx, g2), csh2, csc2)
            tokens = jnp.concatenate([h2.reshape(b * n, d), hc2.reshape(-1, d)], axis=0)
            f = hier_moe(tokens, *moe_p)
            x = x + gt2 * f[:b * n].reshape(b, n, d)
            ctx = ctx + cgt2 * f[b * n:].reshape(b, -1, d)
        else:
            x = x + gt2 * hier_moe(h2.reshape(b * n, d), *moe_p).reshape(b, n, d)
    return rms_norm(x, final_norm_g)
```

```python
import numpy as np
from contextlib import ExitStack
import concourse.bass as bass
import concourse.mybir as mybir
from concourse.bass_utils import run_bass_kernel_spmd

F32, BF16 = mybir.dt.float32, mybir.dt.bfloat16
AF = mybir.ActivationFunctionType
ALU = mybir.AluOpType
AX = mybir.AxisListType

NCORES = 8
D = 1024
KC = 8
SEQ = 16384
TOK = SEQ // NCORES
CTX = 256
DR = 1280
RC = 10
NE = 32
DE = 512
EPS = 1e-6
NDS = 24


class Buf:
    __slots__ = ("name", "w", "r")

    def __init__(self, name):
        self.name = name
        self.w = None
        self.r = {}


class StopBuild(Exception):
    pass


DEBUG_STOP = [None]


class Prog:
    def __init__(self):
        self.nc = bass.Bass("TRN2", target_bir_lowering=False)
        nc = self.nc
        self.es = ExitStack()
        self.eng = {"pe": nc.tensor, "dve": nc.vector, "act": nc.scalar, "pool": nc.gpsimd, "sp": nc.sync}
        self.sem = {k: self.es.enter_context(nc.semaphore("s_" + k)) for k in ("pe", "dve", "act", "pool")}
        self.cnt = {k: 0 for k in self.sem}
        self.waited = {k: {} for k in self.eng}
        self.dsem = [self.es.enter_context(nc.semaphore("d%d" % i)) for i in range(NDS)]
        self.dval = [0] * NDS
        self.dnext = 0
        self.nbuf = 0
        self.out_toks = []
        self.muted = False
        self.es.enter_context(nc.Block())
        self.bank = [self.es.enter_context(nc.psum_tensor("bank%d" % i, [128, 512], F32)) for i in range(8)]
        self.b_bank = [Buf("bank%d" % i) for i in range(8)]

    def buf(self, name="b"):
        self.nbuf += 1
        return Buf("%s%d" % (name, self.nbuf))

    def sb(self, name, shape, dt, scope=None):
        t = (scope or self.es).enter_context(self.nc.sbuf_tensor("t_" + name, list(shape), dt))
        return t

    def ps(self, name, shape, dt=F32, scope=None):
        return (scope or self.es).enter_context(self.nc.psum_tensor(name, list(shape), dt))

    def _wait(self, e, tok):
        if self.muted or tok is None:
            return
        sem, val, key = tok
        if self.waited[e].get(key, 0) >= val:
            return
        self.eng[e].wait_ge(sem, val)
        self.waited[e][key] = val

    def _deps(self, e, reads, writes):
        for b in reads:
            if b.w is not None:
                self._wait(e, b.w)
        for b in writes:
            if b.w is not None:
                self._wait(e, b.w)
            for t in b.r.values():
                self._wait(e, t)

    def _mark(self, tok, reads, writes):
        for b in reads:
            b.r[tok[2]] = tok
        for b in writes:
            b.w = tok
            b.r = {}

    def op(self, e, fn, reads=(), writes=()):
        if self.muted:
            return
        self._deps(e, reads, writes)
        ins = fn(self.eng[e])
        self.cnt[e] += 1
        ins.then_inc(self.sem[e], 1)
        self._mark((self.sem[e], self.cnt[e], e), reads, writes)

    def dma(self, q, out, in_, reads=(), writes=()):
        if self.muted:
            return None
        s = self.dnext
        self.dnext = (s + 1) % NDS
        key = ("d", s)
        if self.dval[s] > 0:
            self._wait(q, (self.dsem[s], self.dval[s], key))
        self._deps(q, reads, writes)
        self.eng[q].dma_start(out=out, in_=in_).then_inc(self.dsem[s], 16)
        self.dval[s] += 16
        tok = (self.dsem[s], self.dval[s], key)
        self._mark(tok, reads, writes)
        return tok

    def barrier(self):
        toks = [(self.sem[k], self.cnt[k], k) for k in self.sem if self.cnt[k] > 0]
        toks += [(self.dsem[i], self.dval[i], ("d", i)) for i in range(NDS) if self.dval[i] > 0]
        for e in self.eng:
            for t in toks:
                self._wait(e, t)

    def checkpoint(self, n):
        if DEBUG_STOP[0] == n and not self.muted:
            self.finish()
            self.muted = True

    def finish(self):
        for t in self.out_toks:
            self._wait("sp", t)
        self.barrier()


class Common:
    def __init__(self, P, ident_d):
        self.P = P
        nc = P.nc
        self.ident = P.sb("ident", [128, 128], F32)
        self.b_ident = P.buf("ident")
        P.dma("sp", self.ident[:], ident_d, writes=[self.b_ident])
        self.ones = P.sb("ones", [128, 128], F32)
        self.b_ones = P.buf("ones")
        P.op("dve", lambda e: e.memset(self.ones[:], 1.0), writes=[self.b_ones])
        self.epsc = P.sb("epsc", [128, 1], F32)
        self.b_eps = P.buf("eps")
        P.op("dve", lambda e: e.memset(self.epsc[:], EPS), writes=[self.b_eps])
        self.vt_in = P.sb("vt_in", [128, 128], F32)
        self.b_vt_in = P.buf("vt_in")
        self.vt_ps = P.bank[0]
        self.b_vt_ps = P.b_bank[0]

    def vecT(self, dram2d, R, dst, b_dst, col0=0):
        P = self.P
        r0 = 0
        while r0 < R:
            n = min(128, R - r0)
            P.dma("sp", self.vt_in[0:n, :], dram2d[r0:r0 + n, :], writes=[self.b_vt_in])
            P.op("pe", lambda e, n=n: e.transpose(self.vt_ps[:, 0:n], self.vt_in[0:n, :], self.ident[0:n, 0:n]),
                 reads=[self.b_vt_in, self.b_ident], writes=[self.b_vt_ps])
            P.op("dve", lambda e, n=n, r0=r0: e.tensor_copy(out=dst[:, col0 + r0:col0 + r0 + n], in_=self.vt_ps[:, 0:n]),
                 reads=[self.b_vt_ps], writes=[b_dst])
            r0 += n


def adaln(P, C, cc_d, ada_w_d, ada_b_d, name):
    sc = P.sb(name + "_sc", [128, 2, 8], F32)
    b_sc = P.buf("sc")
    C.vecT(cc_d.rearrange("r (k p) -> (r k) p", p=128), 16, sc[:].rearrange("p r k -> p (r k)"), b_sc)
    P.op("act", lambda e: e.activation(out=sc[:], in_=sc[:], func=AF.Silu), reads=[b_sc], writes=[b_sc])
    adb = P.sb(name + "_adb", [128, 48], F32)
    b_adb = P.buf("adb")
    C.vecT(ada_b_d.rearrange("(r p) -> r p", p=128), 48, adb[:], b_adb)
    mod = P.sb(name + "_mod", [128, 2, 6, 8], F32)
    b_mod = P.buf("mod")
    mps = P.bank[1][:, 0:96].rearrange("p (m o r) -> p m o r", m=6, o=8, r=2)
    b_mps = P.b_bank[1]
    with ExitStack() as sc_:
        wt = P.sb(name + "_adaw", [128, 8, 1024], F32, scope=sc_)
        b_wt = P.buf("adaw")
        wv = ada_w_d.rearrange("(k p) n -> p k n", p=128)
        for m in range(6):
            P.dma("sp", wt[:], wv[:, :, m * 1024:(m + 1) * 1024], writes=[b_wt])
            for oc in range(8):
                def mm(e, m=m, oc=oc):
                    ins = None
                    for k in range(8):
                        ins = e.matmul(mps[:, m, oc, :], lhsT=wt[:, k, oc * 128:(oc + 1) * 128], rhs=sc[:, :, k],
                                       start=(k == 0), stop=(k == 7))
                    return ins
                P.op("pe", mm, reads=[b_wt, b_sc], writes=[b_mps])
        P.barrier()
    for r in range(2):
        P.op("dve", lambda e, r=r: e.tensor_tensor(out=mod[:, r], in0=mps[:, :, :, r],
                                                   in1=adb[:].rearrange("p (m o) -> p m o", m=6), op=ALU.add),
             reads=[b_mps, b_adb], writes=[b_mod])
    return mod, b_mod


def load_tokens_T(P, C, rows_d, n, dst, b_dst, col0, xt_tiles, b_xt, tps, b_tps, tcount):
    t0 = 0
    while t0 < n:
        w = min(128, n - t0)
        i = tcount[0] % 2
        tcount[0] += 1
        P.dma("sp", xt_tiles[i][0:w, :], rows_d[t0:t0 + w, :], writes=[b_xt[i]])
        for half in range(2):
            def tr(e, i=i, w=w, half=half):
                ins = None
                for q in range(4):
                    kc = half * 4 + q
                    ins = e.transpose(tps[half][:, q * 128:q * 128 + w], xt_tiles[i][0:w, kc * 128:(kc + 1) * 128],
                                      C.ident[0:w, 0:w])
                return ins
            P.op("pe", tr, reads=[b_xt[i], C.b_ident], writes=[b_tps[half]])
            src = tps[half][:].rearrange("p (q t) -> p q t", q=4)[:, :, 0:w]
            eng = "act" if half == 0 else "dve"
            if eng == "act":
                P.op("act", lambda e, half=half, src=src, t0=t0, w=w: e.activation(
                    out=dst[:, half * 4:half * 4 + 4, col0 + t0:col0 + t0 + w], in_=src, func=AF.Copy),
                    reads=[b_tps[half]], writes=[b_dst])
            else:
                P.op("dve", lambda e, half=half, src=src, t0=t0, w=w: e.tensor_copy(
                    out=dst[:, half * 4:half * 4 + 4, col0 + t0:col0 + t0 + w], in_=src),
                    reads=[b_tps[half]], writes=[b_dst])
        t0 += w


def norm_fm(P, C, src, b_src, c0, W, sq, b_sq, ssps, b_ssps, rstd, b_rstd, scale_ap, bias_ap, mod_bufs, outs):
    P.op("act", lambda e: e.activation(out=sq[:, :, 0:W], in_=src[:, :, c0:c0 + W], func=AF.Square),
         reads=[b_src], writes=[b_sq])

    def mm(e):
        ins = None
        for k in range(8):
            ins = e.matmul(ssps[:, 0:W], lhsT=C.ones[:, :], rhs=sq[:, k, 0:W], start=(k == 0), stop=(k == 7))
        return ins
    P.op("pe", mm, reads=[b_sq, C.b_ones], writes=[b_ssps])
    P.op("act", lambda e: e.activation(out=rstd[:, 0:W], in_=ssps[:, 0:W], func=AF.Ln, scale=1.0 / D, bias=C.epsc[:, 0:1]),
         reads=[b_ssps, C.b_eps], writes=[b_rstd])
    P.op("act", lambda e: e.activation(out=rstd[:, 0:W], in_=rstd[:, 0:W], func=AF.Exp, scale=-0.5),
         reads=[b_rstd], writes=[b_rstd])
    P.op("dve", lambda e: e.tensor_tensor(out=sq[:, :, 0:W], in0=src[:, :, c0:c0 + W],
                                          in1=rstd[:, 0:W].unsqueeze(1).to_broadcast([128, 8, W]), op=ALU.mult),
         reads=[b_src, b_rstd], writes=[b_sq])
    for (ot, b_ot, oc0) in outs:
        for k in range(8):
            P.op("act", lambda e, k=k, ot=ot, oc0=oc0: e.activation(
                out=ot[:, k, oc0:oc0 + W], in_=sq[:, k, 0:W], func=AF.Identity,
                scale=scale_ap(k), bias=bias_ap(k)), reads=[b_sq] + mod_bufs, writes=[b_ot])


def moe(P, C, sc, xT, b_xT, groups, gmods, wr_d, br_d, sel_d, wg_d, wu_d, wd_d, T):
    nc = P.nc
    rs = ExitStack()
    h2T = P.sb("h2T", [128, 8, T], BF16, scope=sc)
    b_h2T = P.buf("h2T")
    GT = P.sb("GT", [32, T], F32, scope=sc)
    b_GT = P.buf("GT")
    Gbc = P.sb("Gbc", [128, T], F32, scope=sc)
    b_Gbc = P.buf("Gbc")
    sel = P.sb("sel", [32, 32, 128], F32, scope=sc)
    b_sel = P.buf("sel")
    sq = P.sb("m_sq", [128, 8, 512], F32, scope=rs)
    b_sq = P.buf("m_sq")
    rstd = P.sb("m_rstd", [128, 512], F32, scope=rs)
    b_rstd = P.buf("m_rstd")
    wr = P.sb("wr", [128, 8, 36], F32, scope=rs)
    b_wr = P.buf("wr")
    brow = P.sb("brow", [1, 36], F32, scope=rs)
    b_brow = P.buf("brow")
    wrv = wr_d.rearrange("(k p) n -> p k n", p=128)
    for k in range(8):
        P.dma("sp", wr[:, k, :], wrv[:, k, :], writes=[b_wr])
    P.dma("sp", brow[:], br_d, writes=[b_brow])
    P.dma("sp", sel[:], sel_d, writes=[b_sel])
    bidx = [0, 4, 5, 6, 7, 2, 3]
    pbank = [P.bank[i] for i in bidx]
    b_pbank = [P.b_bank[i] for i in bidx]
    L = P.sb("r_L", [128, 36], F32, scope=rs)
    top = P.sb("r_top", [128, 4, 8], F32, scope=rs)
    small = P.sb("r_small", [128, 32], F32, scope=rs)
    selm = P.sb("r_selm", [128, 8], F32, scope=rs)
    ee = P.sb("r_ee", [128, 8], F32, scope=rs)
    G = P.sb("r_G", [128, 4, 8], F32, scope=rs)
    b_r = P.buf("router")
    for (c0, W, r) in groups:
        scale_ap, bias_ap, gate_ap, mbufs = gmods(r)
        norm_fm(P, C, xT, b_xT, c0, W, sq, b_sq, pbank[0], b_pbank[0], rstd, b_rstd, scale_ap, bias_ap, mbufs, [])
        for k in range(8):
            P.op("act", lambda e, k=k: e.activation(out=sq[:, k, 0:W], in_=sq[:, k, 0:W], func=AF.Identity,
                                                    scale=scale_ap(k), bias=bias_ap(k)), reads=[b_sq] + mbufs, writes=[b_sq])
        P.op("dve", lambda e: e.tensor_copy(out=h2T[:, :, c0:c0 + W], in_=sq[:, :, 0:W]), reads=[b_sq], writes=[b_h2T])
        for t0 in range(0, W, 128):
            lp = pbank[1]

            def mm(e, t0=t0):
                for k in range(8):
                    e.matmul(lp[:, 0:36], lhsT=sq[:, k, t0:t0 + 128], rhs=wr[:, k, :], start=(k == 0), stop=False)
                return e.matmul(lp[:, 0:36], lhsT=C.ones[0:1, :], rhs=brow[0:1, :], start=False, stop=True)
            P.op("pe", mm, reads=[b_sq, b_wr, b_brow, C.b_ones], writes=[b_pbank[1]])
            P.op("dve", lambda e: e.tensor_copy(out=L[:], in_=lp[:, 0:36]), reads=[b_pbank[1]], writes=[b_r])
            P.op("dve", lambda e: e.tensor_reduce(out=small[:, 0:1], in_=L[:, 0:4], axis=AX.X, op=ALU.max), reads=[b_r], writes=[b_r])
            P.op("dve", lambda e: e.tensor_scalar(out=small[:, 1:2], in0=small[:, 0:1], scalar1=-1.0, scalar2=None, op0=ALU.mult),
                 reads=[b_r], writes=[b_r])
            P.op("act", lambda e: e.activation(out=ee[:, 0:4], in_=L[:, 0:4], func=AF.Exp, bias=small[:, 1:2], scale=1.0),
                 reads=[b_r], writes=[b_r])
            P.op("dve", lambda e: e.tensor_reduce(out=small[:, 2:3], in_=ee[:, 0:4], axis=AX.X, op=ALU.add), reads=[b_r], writes=[b_r])
            P.op("dve", lambda e: e.reciprocal(out=small[:, 3:4], in_=small[:, 2:3]), reads=[b_r], writes=[b_r])
            P.op("dve", lambda e: e.tensor_scalar(out=small[:, 4:8], in0=L[:, 0:4], scalar1=small[:, 0:1], scalar2=None,
                                                  op0=ALU.is_equal), reads=[b_r], writes=[b_r])
            for g in range(4):
                P.op("dve", lambda e, g=g: e.max(out=top[:, g, :], in_=L[:, 4 + 8 * g:12 + 8 * g]), reads=[b_r], writes=[b_r])
            P.op("dve", lambda e: e.tensor_scalar(out=small[:, 8:12], in0=top[:, :, 0], scalar1=-1.0, scalar2=None, op0=ALU.mult),
                 reads=[b_r], writes=[b_r])
            for g in range(4):
                P.op("dve", lambda e, g=g: e.tensor_scalar(out=selm[:], in0=L[:, 4 + 8 * g:12 + 8 * g], scalar1=top[:, g, 1:2],
                                                           scalar2=None, op0=ALU.is_ge), reads=[b_r], writes=[b_r])
                P.op("act", lambda e, g=g: e.activation(out=ee[:], in_=L[:, 4 + 8 * g:12 + 8 * g], func=AF.Exp,
                                                        bias=small[:, 8 + g:9 + g], scale=1.0), reads=[b_r], writes=[b_r])
                P.op("dve", lambda e, g=g: e.tensor_tensor(out=G[:, g, :], in0=selm[:], in1=ee[:], op=ALU.mult), reads=[b_r], writes=[b_r])
                P.op("dve", lambda e, g=g: e.tensor_reduce(out=small[:, 12 + g:13 + g], in_=G[:, g, :], axis=AX.X, op=ALU.add),
                     reads=[b_r], writes=[b_r])
            P.op("dve", lambda e: e.reciprocal(out=small[:, 16:20], in_=small[:, 12:16]), reads=[b_r], writes=[b_r])
            P.op("dve", lambda e: e.tensor_tensor(out=small[:, 16:20], in0=small[:, 16:20], in1=small[:, 4:8], op=ALU.mult),
                 reads=[b_r], writes=[b_r])
            P.op("dve", lambda e: e.tensor_scalar(out=small[:, 16:20], in0=small[:, 16:20], scalar1=small[:, 3:4], scalar2=None,
                                                  op0=ALU.mult), reads=[b_r], writes=[b_r])
            P.op("dve", lambda e: e.tensor_tensor(out=G[:], in0=G[:], in1=small[:, 16:20].unsqueeze(2).to_broadcast([128, 4, 8]),
                                                  op=ALU.mult), reads=[b_r], writes=[b_r])
            gp = pbank[2]
            P.op("pe", lambda e: e.transpose(gp[0:32, 0:128], G[:].rearrange("p g e -> p (g e)"), C.ident[:, :]),
                 reads=[b_r, C.b_ident], writes=[b_pbank[2]])
            P.op("act", lambda e, t0=t0: e.activation(out=GT[:, c0 + t0:c0 + t0 + 128], in_=gp[0:32, 0:128], func=AF.Copy),
                 reads=[b_pbank[2]], writes=[b_GT])
    P.barrier()
    rs.close()
    wg = P.sb("wg", [128, 8, DE], BF16, scope=sc)
    wu = P.sb("wu", [128, 8, DE], BF16, scope=sc)
    wd = P.sb("wd", [128, 4, D], BF16, scope=sc)
    b_wg, b_wu, b_wd = P.buf("wg"), P.buf("wu"), P.buf("wd")
    sg = [P.sb("sg%d" % i, [128, 512], F32, scope=sc) for i in range(2)]
    b_sg = [P.buf("sg") for _ in range(2)]
    tt = [P.sb("tt%d" % i, [128, 512], F32, scope=sc) for i in range(2)]
    b_tt = [P.buf("tt") for _ in range(2)]
    act = [P.sb("act%d" % i, [128, 4, 512], BF16, scope=sc) for i in range(2)]
    b_act = [[P.buf("act") for _ in range(4)] for _ in range(2)]
    gu_banks = [(1, 2), (3, 4)]
    o_banks = [5, 6]
    it = 0
    oit = 0
    ait = 0
    for ex in range(NE):
        P.dma("pool", wg[:], wg_d[ex].rearrange("(k p) n -> p k n", p=128), writes=[b_wg])
        P.dma("pool", wu[:], wu_d[ex].rearrange("(k p) n -> p k n", p=128), writes=[b_wu])
        P.dma("pool", wd[:], wd_d[ex].rearrange("(k p) n -> p k n", p=128), writes=[b_wd])
        for (c0, W, r) in groups:
            P.op("pe", lambda e, c0=c0, W=W, ex=ex: e.matmul(pbank[0][:, 0:W], lhsT=sel[:, ex, :], rhs=GT[:, c0:c0 + W],
                                                            start=True, stop=True), reads=[b_sel, b_GT], writes=[b_pbank[0]])
            P.op("act", lambda e, c0=c0, W=W: e.activation(out=Gbc[:, c0:c0 + W], in_=pbank[0][:, 0:W], func=AF.Copy),
                 reads=[b_pbank[0]], writes=[b_Gbc])
        for (c0, W, r) in groups:
            scale_ap, bias_ap, gate_ap, mbufs = gmods(r)
            ai = ait % 2
            ait += 1
            for dc in range(4):
                gi, ui = gu_banks[it % 2]
                si = it % 2
                it += 1

                def mmg(e, dc=dc, gi=gi, c0=c0, W=W):
                    ins = None
                    for k in range(8):
                        ins = e.matmul(pbank[gi][:, 0:W], lhsT=wg[:, k, dc * 128:(dc + 1) * 128], rhs=h2T[:, k, c0:c0 + W],
                                       start=(k == 0), stop=(k == 7))
                    return ins

                def mmu(e, dc=dc, ui=ui, c0=c0, W=W):
                    ins = None
                    for k in range(8):
                        ins = e.matmul(pbank[ui][:, 0:W], lhsT=wu[:, k, dc * 128:(dc + 1) * 128], rhs=h2T[:, k, c0:c0 + W],
                                       start=(k == 0), stop=(k == 7))
                    return ins
                P.op("pe", mmg, reads=[b_wg, b_h2T], writes=[b_pbank[gi]])
                P.op("pe", mmu, reads=[b_wu, b_h2T], writes=[b_pbank[ui]])
                P.op("act", lambda e, gi=gi, si=si, W=W: e.activation(out=sg[si][:, 0:W], in_=pbank[gi][:, 0:W], func=AF.Silu),
                     reads=[b_pbank[gi]], writes=[b_sg[si]])
                P.op("dve", lambda e, ui=ui, si=si, W=W: e.tensor_tensor(out=tt[si][:, 0:W], in0=pbank[ui][:, 0:W],
                                                                         in1=sg[si][:, 0:W], op=ALU.mult),
                     reads=[b_pbank[ui], b_sg[si]], writes=[b_tt[si]])
                P.op("pool", lambda e, si=si, ai=ai, dc=dc, c0=c0, W=W: e.tensor_tensor(
                    out=act[ai][:, dc, 0:W], in0=tt[si][:, 0:W], in1=Gbc[:, c0:c0 + W], op=ALU.mult),
                    reads=[b_tt[si], b_Gbc], writes=[b_act[ai][dc]])
            for oc in range(8):
                ob = o_banks[oit % 2]
                oit += 1

                def mmo(e, oc=oc, ob=ob, ai=ai, W=W):
                    ins = None
                    for dc in range(4):
                        ins = e.matmul(pbank[ob][:, 0:W], lhsT=wd[:, dc, oc * 128:(oc + 1) * 128], rhs=act[ai][:, dc, 0:W],
                                       start=(dc == 0), stop=(dc == 3))
                    return ins
                P.op("pe", mmo, reads=[b_wd] + b_act[ai], writes=[b_pbank[ob]])
                P.op("dve", lambda e, oc=oc, ob=ob, c0=c0, W=W: e.scalar_tensor_tensor(
                    out=xT[:, oc, c0:c0 + W], in0=pbank[ob][:, 0:W], scalar=gate_ap(oc), in1=xT[:, oc, c0:c0 + W],
                    op0=ALU.mult, op1=ALU.add), reads=[b_pbank[ob], b_xT] + mbufs, writes=[b_xT])


def store_tokens(P, C, xT, b_xT, c0, n, rows_d, tps, b_tps, ot, b_ot, b_out, tcount):
    for t0 in range(0, n, 128):
        i = tcount[0] % 2
        tcount[0] += 1
        for half in range(2):
            def tr(e, half=half, t0=t0):
                ins = None
                for q in range(4):
                    kc = half * 4 + q
                    ins = e.transpose(tps[half][:, q * 128:(q + 1) * 128], xT[:, kc, c0 + t0:c0 + t0 + 128], C.ident[:, :])
                return ins
            P.op("pe", tr, reads=[b_xT, C.b_ident], writes=[b_tps[half]])
            if half == 0:
                P.op("act", lambda e, i=i: e.activation(out=ot[i][:, 0:512], in_=tps[0][:, :], func=AF.Copy),
                     reads=[b_tps[0]], writes=[b_ot[i]])
            else:
                P.op("dve", lambda e, i=i: e.tensor_copy(out=ot[i][:, 512:1024], in_=tps[1][:, :]),
                     reads=[b_tps[1]], writes=[b_ot[i]])
        P.out_toks.append(P.dma("sp", rows_d[t0:t0 + 128, :], ot[i][:, :], reads=[b_ot[i]], writes=[b_out]))


def build_l0(pass2):
    P = Prog()
    nc = P.nc

    def din(name, shape):
        return nc.dram_tensor(name, list(shape), F32, kind="ExternalInput").ap()

    x_d = din("x", [TOK, D])
    xh_d = din("xh", [3, D])
    hmask_d = din("hmask", [128, 3])
    ctx_d = din("ctx", [CTX, D])
    cc_d = din("cc", [2, D])
    ident_d = din("ident", [128, 128])
    ada_w_d = din("ada_w", [D, 6 * D])
    ada_b_d = din("ada_b", [6 * D])
    g1_d = din("g1", [D])
    w_in_d = din("w_in", [D, 2 * DR])
    conv_w_d = din("conv_w", [4, DR])
    conv_b_d = din("conv_b", [DR])
    w_a_d = din("w_a", [2, RC, 128, 128])
    b_a_d = din("b_a", [2, DR])
    w_x_d = din("w_x", [2, RC, 128, 128])
    b_x_d = din("b_x", [2, DR])
    lam_d = din("lam", [2, DR])
    if pass2:
        g2_d = din("g2", [D])
        w_out_d = din("w_out", [DR, D])
        st_d = din("st_all", [NCORES * 4 * RC, 128])
        cm_d = din("cmask", [128, 16])
        wr_d = din("wr", [D, 36])
        br_d = din("br", [1, 36])
        sel_d = din("sel", [32, 32, 128])
        wg_d = din("wg", [NE, D, DE])
        wu_d = din("wu", [NE, D, DE])
        wd_d = din("wd", [NE, DE, D])
        x2_d = nc.dram_tensor("x2", [TOK, D], F32, kind="ExternalOutput").ap()
        ctx2_d = nc.dram_tensor("ctx2", [CTX, D], F32, kind="ExternalOutput").ap()
    else:
        st_out_d = nc.dram_tensor("st_out", [4 * RC, 128], F32, kind="ExternalOutput").ap()
    b_out = P.buf("out")

    C = Common(P, ident_d)
    mod, b_mod = adaln(P, C, cc_d, ada_w_d, ada_b_d, "l0")
    sv = P.sb("sv", [128, 160], F32)
    b_sv = P.buf("sv")
    C.vecT(g1_d.rearrange("(r p) -> r p", p=128), 8, sv[:], b_sv, 0)
    if pass2:
        C.vecT(g2_d.rearrange("(r p) -> r p", p=128), 8, sv[:], b_sv, 8)
    C.vecT(conv_w_d.rearrange("j (c p) -> (j c) p", p=128), 40, sv[:], b_sv, 16)
    C.vecT(conv_b_d.rearrange("(c p) -> c p", p=128), 10, sv[:], b_sv, 56)
    C.vecT(b_a_d.rearrange("d (c p) -> (d c) p", p=128), 20, sv[:], b_sv, 66)
    C.vecT(b_x_d.rearrange("d (c p) -> (d c) p", p=128), 20, sv[:], b_sv, 86)
    C.vecT(lam_d.rearrange("d (c p) -> (d c) p", p=128), 20, sv[:], b_sv, 106)
    P.op("act", lambda e: e.activation(out=sv[:, 126:146], in_=sv[:, 106:126], func=AF.Exp, scale=-1.0), reads=[b_sv], writes=[b_sv])
    P.op("act", lambda e: e.activation(out=sv[:, 126:146], in_=sv[:, 126:146], func=AF.Ln, bias=C.ones[:, 0:1], scale=1.0),
         reads=[b_sv, C.b_ones], writes=[b_sv])
    P.op("dve", lambda e: e.tensor_scalar(out=sv[:, 126:146], in0=sv[:, 126:146], scalar1=-8.0, scalar2=None, op0=ALU.mult),
         reads=[b_sv], writes=[b_sv])
    hmask = P.sb("hmask", [128, 3], F32)
    b_hmask = P.buf("hmask")
    P.dma("sp", hmask[:], hmask_d, writes=[b_hmask])
    gm = P.sb("gm", [128, 2, 2, 8], F32)
    b_gm = P.buf("gm")
    for r in range(2):
        for wh in range(2 if pass2 else 1):
            P.op("dve", lambda e, r=r, wh=wh: e.scalar_tensor_tensor(
                out=gm[:, r, wh, :], in0=mod[:, r, 1 + 3 * wh, :], scalar=1.0, in1=sv[:, 8 * wh:8 * wh + 8],
                op0=ALU.add, op1=ALU.mult), reads=[b_mod, b_sv], writes=[b_gm])

    NH = TOK + 3 + CTX
    TT = TOK + CTX
    xt_tiles = [P.sb("xt%d" % i, [128, D], F32) for i in range(2)]
    b_xt = [P.buf("xt") for _ in range(2)]
    sc_mT = ExitStack()
    sc_hT = ExitStack()
    if pass2:
        mT = P.sb("mT", [128, RC, TT], BF16, scope=sc_mT)
        b_mT = P.buf("mT")
    hT = P.sb("hT", [128, 8, NH], BF16, scope=sc_hT)
    b_hT = P.buf("hT")
    tps = [P.bank[2], P.bank[3]]
    b_tps = [P.b_bank[2], P.b_bank[3]]
    tcount = [0]
    with ExitStack() as s1:
        xg = P.sb("xg", [128, 8, 512], F32, scope=s1)
        b_xg = P.buf("xg")
        sq = P.sb("sq", [128, 8, 512], F32, scope=s1)
        b_sq = P.buf("sq")
        rstd = P.sb("rstd", [128, 512], F32, scope=s1)
        b_rstd = P.buf("rstd")
        ssps = P.bank[0]
        b_ssps = P.b_bank[0]
        segs = [(x_d[g * 512:(g + 1) * 512, :], 512, g * 512, 0) for g in range(4)]
        segs.append((xh_d, 3, TOK, 0))
        segs.append((ctx_d, CTX, TOK + 3, 1))
        for (rows, n, hc0, r) in segs:
            load_tokens_T(P, C, rows, n, xg, b_xg, 0, xt_tiles, b_xt, tps, b_tps, tcount)
            norm_fm(P, C, xg, b_xg, 0, n, sq, b_sq, ssps, b_ssps, rstd, b_rstd,
                    lambda k, r=r: gm[:, r, 0, k:k + 1], lambda k, r=r: mod[:, r, 0, k:k + 1], [b_gm, b_mod],
                    [(hT, b_hT, hc0)])
        P.barrier()

    NP_ = TOK + 3 + CTX + 3
    sc_w = ExitStack()
    prec = P.sb("prec", [128, NP_], F32, scope=sc_w)
    u = P.sb("u", [128, TT], F32, scope=sc_w)
    ub = P.sb("ub", [128, TT], BF16, scope=sc_w)
    A = P.sb("A", [128, TT], F32, scope=sc_w)
    Bb = P.sb("Bb", [128, TT], F32, scope=sc_w)
    Tt = P.sb("Tt", [128, TT], F32, scope=sc_w)
    HF = P.sb("HF", [128, TT], F32, scope=sc_w)
    HB = P.sb("HB", [128, TT], F32, scope=sc_w)
    b_prec, b_u, b_ub, b_A, b_Bb, b_Tt, b_HF, b_HB = [P.buf(n) for n in ("prec", "u", "ub", "A", "Bb", "Tt", "HF", "HB")]
    P.op("dve", lambda e: e.memset(prec[:, TOK + 3:NP_], 0.0), writes=[b_prec])
    wch = P.sb("wch", [128, 8, 256], BF16, scope=sc_w)
    b_wch = P.buf("wch")
    wgt = P.sb("wgt", [128, 4, 128], BF16, scope=sc_w)
    b_wgt = P.buf("wgt")
    racc = P.sb("racc", [128, 2, 4], F32, scope=sc_w)
    b_racc = P.buf("racc")
    st = P.sb("st", [128, 4, RC], F32, scope=sc_w)
    b_st = P.buf("st")
    hin = P.sb("hin", [128, 2], F32, scope=sc_w)
    b_hin = P.buf("hin")
    pb = [P.bank[4 + i] for i in range(4)]
    b_pb = [P.b_bank[4 + i] for i in range(4)]
    if pass2:
        Gg = P.sb("Gg", [128, TT], F32, scope=sc_w)
        b_Gg = P.buf("Gg")
        stall = P.sb("stall", [128, NCORES, 4, RC], F32, scope=sc_w)
        b_stall = P.buf("stall")
        C.vecT(st_d, NCORES * 4 * RC, stall[:].rearrange("p a b c -> p (a b c)"), b_stall)
        cm = P.sb("cm", [128, 16], F32, scope=sc_w)
        b_cm = P.buf("cm")
        P.dma("sp", cm[:], cm_d, writes=[b_cm])
        for i in range(NCORES):
            for d in range(2):
                mcol = cm[:, d * 8 + i:d * 8 + i + 1]
                P.op("dve", lambda e, i=i, d=d: e.tensor_scalar(
                    out=stall[:, i, 2 * d, :], in0=stall[:, i, 2 * d, :], scalar1=-1.0, scalar2=None, op0=ALU.add),
                    reads=[b_stall], writes=[b_stall])
                P.op("dve", lambda e, i=i, d=d, mcol=mcol: e.tensor_scalar(
                    out=stall[:, i, 2 * d, :], in0=stall[:, i, 2 * d, :], scalar1=mcol, scalar2=None, op0=ALU.mult),
                    reads=[b_stall, b_cm], writes=[b_stall])
                P.op("dve", lambda e, i=i, d=d: e.tensor_scalar(
                    out=stall[:, i, 2 * d, :], in0=stall[:, i, 2 * d, :], scalar1=1.0, scalar2=None, op0=ALU.add),
                    reads=[b_stall], writes=[b_stall])
                P.op("dve", lambda e, i=i, d=d, mcol=mcol: e.tensor_scalar(
                    out=stall[:, i, 2 * d + 1, :], in0=stall[:, i, 2 * d + 1, :], scalar1=mcol, scalar2=None, op0=ALU.mult),
                    reads=[b_stall, b_cm], writes=[b_stall])
    groups = [(g * 512, 512) for g in range(4)] + [(TOK, CTX)]
    w_in_v = w_in_d.rearrange("(k p) n -> p k n", p=128)
    pit = 0
    for j in range(RC):
        P.dma("pool", wch[:, :, 0:128], w_in_v[:, :, j * 128:(j + 1) * 128], writes=[b_wch])
        P.dma("pool", wch[:, :, 128:256], w_in_v[:, :, DR + j * 128:DR + (j + 1) * 128], writes=[b_wch])
        for d in range(2):
            P.dma("pool", wgt[:, 2 * d, :], w_a_d[d, j], writes=[b_wgt])
            P.dma("pool", wgt[:, 2 * d + 1, :], w_x_d[d, j], writes=[b_wgt])
        for gi, (c0, W) in enumerate(groups + [(TOK, 3)]):
            halo = (gi == 5)
            hc0 = c0 if gi < 4 else (TOK if halo else TOK + 3)
            bi = pit % 4
            pit += 1

            def mm(e, bi=bi, hc0=hc0, W=W):
                ins = None
                for k in range(8):
                    ins = e.matmul(pb[bi][:, 0:W], lhsT=wch[:, k, 128:256], rhs=hT[:, k, hc0:hc0 + W], start=(k == 0), stop=(k == 7))
                return ins
            P.op("pe", mm, reads=[b_wch, b_hT], writes=[b_pb[bi]])
            if halo:
                P.op("dve", lambda e, bi=bi: e.tensor_tensor(out=prec[:, 0:2], in0=pb[bi][:, 0:2], in1=hmask[:, 0:2], op=ALU.mult),
                     reads=[b_pb[bi], b_hmask], writes=[b_prec])
                P.op("dve", lambda e, bi=bi: e.tensor_tensor(out=prec[:, TOK + 2:TOK + 3], in0=pb[bi][:, 2:3], in1=hmask[:, 2:3], op=ALU.mult),
                     reads=[b_pb[bi], b_hmask], writes=[b_prec])
            else:
                pc0 = 2 + c0 if gi < 4 else TOK + 3 + 2
                P.op("act", lambda e, bi=bi, pc0=pc0, W=W: e.activation(out=prec[:, pc0:pc0 + W], in_=pb[bi][:, 0:W], func=AF.Copy),
                     reads=[b_pb[bi]], writes=[b_prec])
        for (uc0, n, pc0) in ((0, TOK, 0), (TOK, CTX, TOK + 3)):
            P.op("dve", lambda e, uc0=uc0, n=n, pc0=pc0: e.tensor_scalar(
                out=u[:, uc0:uc0 + n], in0=prec[:, pc0:pc0 + n], scalar1=sv[:, 16 + j:17 + j], scalar2=sv[:, 56 + j:57 + j],
                op0=ALU.mult, op1=ALU.add), reads=[b_prec, b_sv], writes=[b_u])
            for jj in range(1, 4):
                P.op("dve", lambda e, uc0=uc0, n=n, pc0=pc0, jj=jj: e.scalar_tensor_tensor(
                    out=u[:, uc0:uc0 + n], in0=prec[:, pc0 + jj:pc0 + jj + n], scalar=sv[:, 16 + jj * 10 + j:17 + jj * 10 + j],
                    in1=u[:, uc0:uc0 + n], op0=ALU.mult, op1=ALU.add), reads=[b_prec, b_sv, b_u], writes=[b_u])
        P.op("act", lambda e: e.activation(out=ub[:, :], in_=u[:, :], func=AF.Copy), reads=[b_u], writes=[b_ub])
        for d in range(2):
            Hd, b_Hd = (HF, b_HF) if d == 0 else (HB, b_HB)
            for gi, (c0, W) in enumerate(groups):
                b1 = pit % 4
                pit += 1
                b2 = pit % 4
                pit += 1
                P.op("pe", lambda e, b1=b1, c0=c0, W=W, d=d: e.matmul(pb[b1][:, 0:W], lhsT=wgt[:, 2 * d, :], rhs=ub[:, c0:c0 + W],
                                                                      start=True, stop=True), reads=[b_wgt, b_ub], writes=[b_pb[b1]])
                P.op("pe", lambda e, b2=b2, c0=c0, W=W, d=d: e.matmul(pb[b2][:, 0:W], lhsT=wgt[:, 2 * d + 1, :], rhs=ub[:, c0:c0 + W],
                                                                      start=True, stop=True), reads=[b_wgt, b_ub], writes=[b_pb[b2]])
                P.op("act", lambda e, b1=b1, c0=c0, W=W, d=d: e.activation(
                    out=A[:, c0:c0 + W], in_=pb[b1][:, 0:W], func=AF.Sigmoid, bias=sv[:, 66 + d * 10 + j:67 + d * 10 + j], scale=1.0),
                    reads=[b_pb[b1], b_sv], writes=[b_A])
                P.op("act", lambda e, b2=b2, c0=c0, W=W, d=d: e.activation(
                    out=Bb[:, c0:c0 + W], in_=pb[b2][:, 0:W], func=AF.Sigmoid, bias=sv[:, 86 + d * 10 + j:87 + d * 10 + j], scale=1.0),
                    reads=[b_pb[b2], b_sv], writes=[b_Bb])
            clc = sv[:, 126 + d * 10 + j:127 + d * 10 + j]
            if not pass2:
                P.op("dve", lambda e, d=d: e.tensor_reduce(out=racc[:, d, 0:1], in_=A[:, 0:TOK], axis=AX.X, op=ALU.add),
                     reads=[b_A], writes=[b_racc])
            P.op("act", lambda e, clc=clc: e.activation(out=A[:, :], in_=A[:, :], func=AF.Exp, scale=clc), reads=[b_A, b_sv], writes=[b_A])
            P.op("dve", lambda e: e.tensor_tensor(out=Tt[:, :], in0=A[:, :], in1=A[:, :], op=ALU.mult), reads=[b_A], writes=[b_Tt])
            P.op("act", lambda e: e.activation(out=Tt[:, :], in_=Tt[:, :], func=AF.Sqrt, scale=-1.0, bias=C.ones[:, 0:1]),
                 reads=[b_Tt, C.b_ones], writes=[b_Tt])
            P.op("pool", lambda e: e.tensor_tensor(out=Bb[:, :], in0=Bb[:, :], in1=Tt[:, :], op=ALU.mult), reads=[b_Bb, b_Tt], writes=[b_Bb])
            P.op("dve", lambda e: e.tensor_tensor(out=Bb[:, :], in0=Bb[:, :], in1=u[:, :], op=ALU.mult), reads=[b_Bb, b_u], writes=[b_Bb])
            if d == 0:
                P.op("dve", lambda e: e.tensor_tensor_scan(out=HF[:, TOK:TT], data0=A[:, TOK:TT], data1=Bb[:, TOK:TT], initial=0.0,
                                                           op0=ALU.mult, op1=ALU.add), reads=[b_A, b_Bb], writes=[b_HF])
                cend = HF[:, TT - 1:TT]
            else:
                P.op("dve", lambda e: e.tensor_tensor_scan(out=HB[:, TOK:TT][:, ::-1], data0=A[:, TOK:TT][:, ::-1],
                                                           data1=Bb[:, TOK:TT][:, ::-1], initial=0.0, op0=ALU.mult, op1=ALU.add),
                     reads=[b_A, b_Bb], writes=[b_HB])
                cend = HB[:, TOK:TOK + 1]
            if pass2:
                P.op("dve", lambda e, d=d, cend=cend: e.tensor_copy(out=hin[:, d:d + 1], in_=cend), reads=[b_Hd], writes=[b_hin])
                order = range(NCORES) if d == 0 else range(NCORES - 1, -1, -1)
                for i in order:
                    P.op("dve", lambda e, i=i, d=d: e.scalar_tensor_tensor(
                        out=hin[:, d:d + 1], in0=hin[:, d:d + 1], scalar=stall[:, i, 2 * d, j:j + 1], in1=stall[:, i, 2 * d + 1, j:j + 1],
                        op0=ALU.mult, op1=ALU.add), reads=[b_hin, b_stall], writes=[b_hin])
                init = hin[:, d:d + 1]
                rd = [b_hin]
            else:
                init = 0.0
                rd = []
            if d == 0:
                P.op("dve", lambda e, init=init: e.tensor_tensor_scan(out=HF[:, 0:TOK], data0=A[:, 0:TOK], data1=Bb[:, 0:TOK], initial=init,
                                                                      op0=ALU.mult, op1=ALU.add), reads=[b_A, b_Bb] + rd, writes=[b_HF])
            else:
                P.op("dve", lambda e, init=init: e.tensor_tensor_scan(out=HB[:, 0:TOK][:, ::-1], data0=A[:, 0:TOK][:, ::-1],
                                                                      data1=Bb[:, 0:TOK][:, ::-1], initial=init, op0=ALU.mult, op1=ALU.add),
                     reads=[b_A, b_Bb] + rd, writes=[b_HB])
            if not pass2:
                P.op("act", lambda e, d=d, clc=clc: e.activation(out=st[:, 2 * d, j:j + 1], in_=racc[:, d, 0:1], func=AF.Exp, scale=clc),
                     reads=[b_racc, b_sv], writes=[b_st])
                src = HF[:, TOK - 1:TOK] if d == 0 else HB[:, 0:1]
                P.op("dve", lambda e, d=d, src=src: e.tensor_copy(out=st[:, 2 * d + 1, j:j + 1], in_=src), reads=[b_Hd], writes=[b_st])
        if pass2:
            for gi, (c0, W) in enumerate(groups):
                hc0 = c0 if gi < 4 else TOK + 3
                bi = pit % 4
                pit += 1

                def mm(e, bi=bi, hc0=hc0, W=W):
                    ins = None
                    for k in range(8):
                        ins = e.matmul(pb[bi][:, 0:W], lhsT=wch[:, k, 0:128], rhs=hT[:, k, hc0:hc0 + W], start=(k == 0), stop=(k == 7))
                    return ins
                P.op("pe", mm, reads=[b_wch, b_hT], writes=[b_pb[bi]])
                P.op("act", lambda e, bi=bi, c0=c0, W=W: e.activation(out=Gg[:, c0:c0 + W], in_=pb[bi][:, 0:W], func=AF.Gelu),
                     reads=[b_pb[bi]], writes=[b_Gg])
            P.op("pool", lambda e: e.tensor_tensor(out=HF[:, :], in0=HF[:, :], in1=HB[:, :], op=ALU.add), reads=[b_HF, b_HB], writes=[b_HF])
            P.op("dve", lambda e, j=j: e.tensor_tensor(out=mT[:, j, :], in0=HF[:, :], in1=Gg[:, :], op=ALU.mult),
                 reads=[b_HF, b_Gg], writes=[b_mT])

    if not pass2:
        P.op("pe", lambda e: e.transpose(pb[0][0:40, 0:128], st[:].rearrange("p a c -> p (a c)"), C.ident[:, :]),
             reads=[b_st, C.b_ident], writes=[b_pb[0]])
        P.op("dve", lambda e: e.tensor_copy(out=A[0:40, 0:128], in_=pb[0][0:40, 0:128]), reads=[b_pb[0]], writes=[b_A])
        P.out_toks.append(P.dma("sp", st_out_d, A[0:40, 0:128], reads=[b_A], writes=[b_out]))
        P.finish()
        sc_w.close()
        sc_hT.close()
        P.es.close()
        return nc

    P.barrier()
    sc_w.close()
    sc_hT.close()

    x1s = nc.dram_tensor("x1s", [128, 8, TT], F32).ap()
    b_x1s = P.buf("x1s")
    sc_o = ExitStack()
    wout = P.sb("wout", [128, RC, D], BF16, scope=sc_o)
    b_wout = P.buf("wout")
    P.dma("pool", wout[:], w_out_d.rearrange("(c p) n -> p c n", p=128), writes=[b_wout])
    xo = P.sb("xo", [128, 8, 512], F32, scope=sc_o)
    b_xo = P.buf("xo")
    yps = [P.bank[4], P.bank[5]]
    b_yps = [P.b_bank[4], P.b_bank[5]]
    yi = 0
    for gi, (c0, W) in enumerate(groups):
        r = 0 if gi < 4 else 1
        rows = x_d[c0:c0 + W, :] if gi < 4 else ctx_d
        load_tokens_T(P, C, rows, W, xo, b_xo, 0, xt_tiles, b_xt, tps, b_tps, tcount)
        for oc in range(8):
            bi = yi % 2
            yi += 1

            def mm(e, bi=bi, oc=oc, c0=c0, W=W):
                ins = None
                for jj in range(RC):
                    ins = e.matmul(yps[bi][:, 0:W], lhsT=wout[:, jj, oc * 128:(oc + 1) * 128], rhs=mT[:, jj, c0:c0 + W],
                                   start=(jj == 0), stop=(jj == RC - 1))
                return ins
            P.op("pe", mm, reads=[b_wout, b_mT], writes=[b_yps[bi]])
            P.op("dve", lambda e, bi=bi, oc=oc, W=W, r=r: e.scalar_tensor_tensor(
                out=xo[:, oc, 0:W], in0=yps[bi][:, 0:W], scalar=mod[:, r, 2, oc:oc + 1], in1=xo[:, oc, 0:W],
                op0=ALU.mult, op1=ALU.add), reads=[b_yps[bi], b_xo, b_mod], writes=[b_xo])
        P.dma("sp", x1s[:, :, c0:c0 + W], xo[:, :, 0:W], reads=[b_xo], writes=[b_x1s])
    P.barrier()
    sc_o.close()
    sc_mT.close()
    sc2 = ExitStack()
    xT = P.sb("xT", [128, 8, TT], F32, scope=sc2)
    b_xT = P.buf("xT")
    for k in range(8):
        P.dma("sp", xT[:, k, :], x1s[:, k, :], reads=[b_x1s], writes=[b_xT])
    with ExitStack() as sc3:
        def gmods(r):
            return (lambda k: gm[:, r, 1, k:k + 1], lambda k: mod[:, r, 3, k:k + 1], lambda k: mod[:, r, 5, k:k + 1], [b_gm, b_mod])
        mgroups = [(g * 512, 512, 0) for g in range(4)] + [(TOK, CTX, 1)]
        moe(P, C, sc3, xT, b_xT, mgroups, gmods, wr_d, br_d, sel_d, wg_d, wu_d, wd_d, TT)
        store_tokens(P, C, xT, b_xT, 0, TOK, x2_d, tps, b_tps, xt_tiles, b_xt, b_out, tcount)
        store_tokens(P, C, xT, b_xT, TOK, CTX, ctx2_d, tps, b_tps, xt_tiles, b_xt, b_out, tcount)
        P.finish()
    sc2.close()
    P.es.close()
    return nc


NT1 = 18


def build_l1(pass2):
    P = Prog()
    P.scopes = []
    try:
        return _build_l1(P, pass2)
    except StopBuild:
        return P.nc


def _build_l1(P, pass2):
    nc = P.nc

    def din(name, shape):
        return nc.dram_tensor(name, list(shape), F32, kind="ExternalInput").ap()

    x_d = din("x", [TOK, D])
    ctx_d = din("ctx", [CTX, D])
    cc_d = din("cc", [2, D])
    ident_d = din("ident", [128, 128])
    ada_w_d = din("ada_w", [D, 6 * D])
    ada_b_d = din("ada_b", [6 * D])
    g1_d = din("g1", [D])
    w_in_d = din("w_in", [D, 3104])
    bg_d = din("bg", [1, 32])
    lf_d = din("Lf", [128, 128])
    lb_d = din("Lb", [128, 128])
    esel_d = din("esel", [32, 8, 128])
    if pass2:
        g2_d = din("g2", [D])
        gf_d = din("gf", [D])
        ng_d = din("ng", [1, D])
        w_out_d = din("w_out", [D, D])
        cl_d = din("cl_all", [NCORES, 4, 2, 128, 129])
        dt_d = din("dt_all", [NCORES, 128, 8])
        cm_d = din("cmask", [128, 16])
        wr_d = din("wr", [D, 36])
        br_d = din("br", [1, 36])
        sel_d = din("sel", [32, 32, 128])
        wg_d = din("wg", [NE, D, DE])
        wu_d = din("wu", [NE, D, DE])
        wd_d = din("wd", [NE, DE, D])
        out_d = nc.dram_tensor("out", [TOK, D], F32, kind="ExternalOutput").ap()
    else:
        clo_d = nc.dram_tensor("cl_out", [4, 2, 128, 129], F32, kind="ExternalOutput").ap()
        dto_d = nc.dram_tensor("dt_out", [128, 8], F32, kind="ExternalOutput").ap()
    b_out = P.buf("out")

    C = Common(P, ident_d)
    mod, b_mod = adaln(P, C, cc_d, ada_w_d, ada_b_d, "l1")
    sv = P.sb("sv", [128, 32], F32)
    b_sv = P.buf("sv")
    P.op("dve", lambda e: e.memset(sv[:], 0.0), writes=[b_sv])
    C.vecT(g1_d.rearrange("(r p) -> r p", p=128), 8, sv[:], b_sv, 0)
    if pass2:
        C.vecT(g2_d.rearrange("(r p) -> r p", p=128), 8, sv[:], b_sv, 8)
        C.vecT(gf_d.rearrange("(r p) -> r p", p=128), 8, sv[:], b_sv, 16)
    gm = P.sb("gm", [128, 2, 2, 8], F32)
    b_gm = P.buf("gm")
    for r in range(2):
        for wh in range(2 if pass2 else 1):
            P.op("dve", lambda e, r=r, wh=wh: e.scalar_tensor_tensor(
                out=gm[:, r, wh, :], in0=mod[:, r, 1 + 3 * wh, :], scalar=1.0, in1=sv[:, 8 * wh:8 * wh + 8],
                op0=ALU.add, op1=ALU.mult), reads=[b_mod, b_sv], writes=[b_gm])
    Lf = P.sb("Lf", [128, 128], F32)
    Lb = P.sb("Lb", [128, 128], F32)
    b_L = P.buf("L")
    P.dma("sp", Lf[:], lf_d, writes=[b_L])
    P.dma("sp", Lb[:], lb_d, writes=[b_L])
    Lm = [Lf, Lb]
    esel = P.sb("esel", [32, 8, 128], F32)
    b_esel = P.buf("esel")
    P.dma("sp", esel[:], esel_d, writes=[b_esel])
    bgrow = P.sb("bgrow", [1, 32], F32)
    b_bgrow = P.buf("bgrow")
    P.dma("sp", bgrow[:], bg_d, writes=[b_bgrow])
    bgT = P.sb("bgT", [32, 1], F32)
    b_bgT = P.buf("bgT")
    P.op("pe", lambda e: e.matmul(P.bank[0][0:32, 0:1], lhsT=bgrow[0:1, :], rhs=C.ones[0:1, 0:1], start=True, stop=True),
         reads=[b_bgrow, C.b_ones], writes=[P.b_bank[0]])
    P.op("dve", lambda e: e.tensor_copy(out=bgT[:], in_=P.bank[0][0:32, 0:1]), reads=[P.b_bank[0]], writes=[b_bgT])
    bgbc = P.sb("bgbc", [128, 32], F32)
    b_bgbc = P.buf("bgbc")
    P.op("pe", lambda e: e.matmul(P.bank[0][:, 0:32], lhsT=C.ones[0:1, :], rhs=bgrow[0:1, :], start=True, stop=True),
         reads=[b_bgrow, C.b_ones], writes=[P.b_bank[0]])
    P.op("dve", lambda e: e.tensor_copy(out=bgbc[:], in_=P.bank[0][:, 0:32]), reads=[P.b_bank[0]], writes=[b_bgbc])
    TT = TOK + CTX
    G4 = P.sb("G4", [128, NT1, 32], F32)
    lsm = P.sb("lsm", [128, NT1, 32], F32)
    ccs = P.sb("ccs", [128, NT1, 2, 8], F32)
    ec = P.sb("ec", [128, NT1, 2, 8], F32)
    ew = P.sb("ew", [128, NT1, 2, 8], F32)
    dec = P.sb("dec", [128, 8, NT1], F32)
    csel = P.sb("csel", [128, 8, NT1], F32)
    dtc = P.sb("dtc", [128, 8], F32)
    b_dtc = P.buf("dtc")
    b_G4, b_lsm, b_ccs, b_ec, b_ew, b_dec, b_csel = [P.buf(n) for n in ("G4", "lsm", "ccs", "ec", "ew", "dec", "csel")]
    xt_tiles = [P.sb("xt%d" % i, [128, D], F32) for i in range(2)]
    b_xt = [P.buf("xt") for _ in range(2)]
    tps = [P.bank[2], P.bank[3]]
    b_tps = [P.b_bank[2], P.b_bank[3]]
    tcount = [0]
    if pass2:
        ngbc = P.sb("ngbc", [128, D], F32)
        b_ngbc = P.buf("ngbc")
        ngrow = P.sb("ngrow", [1, D], F32)
        b_ngrow = P.buf("ngrow")
        P.dma("sp", ngrow[:], ng_d, writes=[b_ngrow])
        for hlf in range(2):
            P.op("pe", lambda e, hlf=hlf: e.matmul(P.bank[0][:, 0:512], lhsT=C.ones[0:1, :], rhs=ngrow[0:1, hlf * 512:(hlf + 1) * 512],
                                                   start=True, stop=True), reads=[b_ngrow, C.b_ones], writes=[P.b_bank[0]])
            P.op("dve", lambda e, hlf=hlf: e.tensor_copy(out=ngbc[:, hlf * 512:(hlf + 1) * 512], in_=P.bank[0][:, 0:512]),
                 reads=[P.b_bank[0]], writes=[b_ngbc])
        cm = P.sb("cm", [128, 16], F32)
        b_cm = P.buf("cm")
        P.dma("sp", cm[:], cm_d, writes=[b_cm])
        Dst = P.sb("Dst", [128, NCORES, 8], F32)
        b_Dst = P.buf("Dst")
        P.dma("sp", Dst[:], dt_d.rearrange("i p n -> p i n"), writes=[b_Dst])
        for i in range(NCORES):
            for d in range(2):
                mcol = cm[:, d * 8 + i:d * 8 + i + 1]
                view = Dst[:, i, :].rearrange("p (a d) -> p a d", d=2)[:, :, d]
                P.op("dve", lambda e, view=view: e.tensor_scalar(out=view, in0=view, scalar1=-1.0, scalar2=None, op0=ALU.add),
                     reads=[b_Dst], writes=[b_Dst])
                P.op("dve", lambda e, view=view, mcol=mcol: e.tensor_scalar(out=view, in0=view, scalar1=mcol, scalar2=None, op0=ALU.mult),
                     reads=[b_Dst, b_cm], writes=[b_Dst])
                P.op("dve", lambda e, view=view: e.tensor_scalar(out=view, in0=view, scalar1=1.0, scalar2=None, op0=ALU.add),
                     reads=[b_Dst], writes=[b_Dst])

    P.checkpoint(1)
    sc_hn = ExitStack()
    sc_hT = ExitStack()
    P.scopes += [sc_hn, sc_hT]
    if pass2:
        hnT = P.sb("hnT", [128, 8, TOK], BF16, scope=sc_hn)
        b_hnT = P.buf("hnT")
    hT = P.sb("hT", [128, 8, TT], BF16, scope=sc_hT)
    b_hT = P.buf("hT")
    with ExitStack() as s1:
        xg = P.sb("xg", [128, 8, 512], F32, scope=s1)
        b_xg = P.buf("xg")
        sq = P.sb("sq", [128, 8, 512], F32, scope=s1)
        b_sq = P.buf("sq")
        rstd = P.sb("rstd", [128, 512], F32, scope=s1)
        b_rstd = P.buf("rstd")
        segs = [(x_d[g * 512:(g + 1) * 512, :], 512, g * 512, 0) for g in range(4)]
        segs.append((ctx_d, CTX, TOK, 1))
        for (rows, n, hc0, r) in segs:
            load_tokens_T(P, C, rows, n, xg, b_xg, 0, xt_tiles, b_xt, tps, b_tps, tcount)
            norm_fm(P, C, xg, b_xg, 0, n, sq, b_sq, P.bank[0], P.b_bank[0], rstd, b_rstd,
                    lambda k, r=r: gm[:, r, 0, k:k + 1], lambda k, r=r: mod[:, r, 0, k:k + 1], [b_gm, b_mod],
                    [(hT, b_hT, hc0)])
        P.barrier()

    P.checkpoint(2)
    groups = [(g * 512, 512) for g in range(4)] + [(TOK, CTX)]
    w_in_v = w_in_d.rearrange("(k p) n -> p k n", p=128)
    with ExitStack() as s2:
        wgb = P.sb("wgb", [128, 8, 32], BF16, scope=s2)
        b_wgb = P.buf("wgb")
        P.dma("pool", wgb[:], w_in_v[:, :, 3072:3104], writes=[b_wgb])
        gT = P.sb("gT", [32, TT], F32, scope=s2)
        b_gT = P.buf("gT")
        cend = P.sb("cend", [32, NT1], F32, scope=s2)
        b_cend = P.buf("cend")
        for (c0, W) in groups:
            def mm(e, c0=c0, W=W):
                ins = None
                for k in range(8):
                    ins = e.matmul(P.bank[4][0:32, 0:W], lhsT=wgb[:, k, :], rhs=hT[:, k, c0:c0 + W], start=(k == 0), stop=(k == 7))
                return ins
            P.op("pe", mm, reads=[b_wgb, b_hT], writes=[P.b_bank[4]])
            P.op("act", lambda e, c0=c0, W=W: e.activation(out=gT[:, c0:c0 + W], in_=P.bank[4][0:32, 0:W], func=AF.Identity,
                                                           bias=bgT[:, 0:1], scale=1.0), reads=[P.b_bank[4], b_bgT], writes=[b_gT])
        P.op("act", lambda e: e.activation(out=gT[:], in_=gT[:], func=AF.Exp, scale=-1.0), reads=[b_gT], writes=[b_gT])
        P.op("act", lambda e: e.activation(out=gT[:], in_=gT[:], func=AF.Ln, bias=C.ones[0:32, 0:1], scale=1.0),
             reads=[b_gT, C.b_ones], writes=[b_gT])
        P.op("dve", lambda e: e.tensor_reduce(out=cend[:], in_=gT[:].rearrange("p (t c) -> p t c", c=128), axis=AX.X, op=ALU.add),
             reads=[b_gT], writes=[b_cend])
        P.op("dve", lambda e: e.tensor_scalar(out=cend[:], in0=cend[:], scalar1=-1.0, scalar2=None, op0=ALU.mult),
             reads=[b_cend], writes=[b_cend])
        for pd in range(8):
            P.op("pe", lambda e, pd=pd: e.matmul(P.bank[5][:, 0:NT1], lhsT=esel[:, pd, :], rhs=cend[:], start=True, stop=True),
                 reads=[b_esel, b_cend], writes=[P.b_bank[5]])
            P.op("dve", lambda e, pd=pd: e.tensor_copy(out=csel[:, pd, :], in_=P.bank[5][:, 0:NT1]), reads=[P.b_bank[5]], writes=[b_csel])
        P.op("act", lambda e: e.activation(out=dec[:], in_=csel[:], func=AF.Exp), reads=[b_csel], writes=[b_dec])
        for t in range(NT1):
            bi = 6 + (t % 2)

            def mm(e, t=t, bi=bi):
                ins = None
                for k in range(8):
                    ins = e.matmul(P.bank[bi][:, 0:32], lhsT=hT[:, k, t * 128:(t + 1) * 128], rhs=wgb[:, k, :], start=(k == 0), stop=(k == 7))
                return ins
            P.op("pe", mm, reads=[b_wgb, b_hT], writes=[P.b_bank[bi]])
            P.op("dve", lambda e, t=t, bi=bi: e.tensor_tensor(out=G4[:, t, :], in0=P.bank[bi][:, 0:32], in1=bgbc[:], op=ALU.add),
                 reads=[P.b_bank[bi], b_bgbc], writes=[b_G4])
        P.op("act", lambda e: e.activation(out=lsm[:], in_=G4[:], func=AF.Exp, scale=-1.0), reads=[b_G4], writes=[b_lsm])
        P.op("act", lambda e: e.activation(out=lsm[:], in_=lsm[:], func=AF.Ln, bias=C.ones[:, 0:1], scale=1.0),
             reads=[b_lsm, C.b_ones], writes=[b_lsm])
        P.op("dve", lambda e: e.tensor_scalar(out=lsm[:], in0=lsm[:], scalar1=-1.0, scalar2=None, op0=ALU.mult), reads=[b_lsm], writes=[b_lsm])
        for t in range(NT1):
            bi = 6 + (t % 2)
            P.op("pe", lambda e, t=t, bi=bi: e.matmul(P.bank[bi][:, 0:8], lhsT=Lf[:, :], rhs=lsm[:, t, 8:16], start=True, stop=True),
                 reads=[b_L, b_lsm], writes=[P.b_bank[bi]])
            P.op("pe", lambda e, t=t, bi=bi: e.matmul(P.bank[bi][:, 8:16], lhsT=Lb[:, :], rhs=lsm[:, t, 24:32], start=True, stop=True),
                 reads=[b_L, b_lsm], writes=[P.b_bank[bi]])
            P.op("dve", lambda e, t=t, bi=bi: e.tensor_copy(out=ccs[:, t].rearrange("p d h -> p (d h)"), in_=P.bank[bi][:, 0:16]),
                 reads=[P.b_bank[bi]], writes=[b_ccs])
        P.op("act", lambda e: e.activation(out=ec[:], in_=ccs[:], func=AF.Exp), reads=[b_ccs], writes=[b_ec])
        liv = G4[:].rearrange("p t (k h) -> p t k h", k=4)[:, :, 0::2, :]
        P.op("dve", lambda e: e.tensor_tensor(out=ew[:], in0=liv, in1=ccs[:], op=ALU.subtract), reads=[b_G4, b_ccs], writes=[b_ew])
        P.op("act", lambda e: e.activation(out=ew[:], in_=ew[:], func=AF.Exp), reads=[b_ew], writes=[b_ew])
        P.barrier()

    P.checkpoint(3)
    own_tiles = list(range(16))
    for hp in range(4):
        with ExitStack() as sp_:
            wk = P.sb("wk%d" % hp, [128, 8, 128], BF16, scope=sp_)
            wv = P.sb("wv%d" % hp, [128, 8, 256], BF16, scope=sp_)
            b_wk, b_wv = P.buf("wk"), P.buf("wv")
            P.dma("pool", wk[:], w_in_v[:, :, 512 + hp * 128:512 + (hp + 1) * 128], writes=[b_wk])
            P.dma("pool", wv[:], w_in_v[:, :, 1024 + hp * 256:1024 + (hp + 1) * 256], writes=[b_wv])
            k_tm = P.sb("k_tm%d" % hp, [128, NT1, 128], BF16, scope=sp_)
            v_tm = P.sb("v_tm%d" % hp, [128, NT1, 256], BF16, scope=sp_)
            b_ktm, b_vtm = P.buf("ktm"), P.buf("vtm")
            C32 = P.sb("C32_%d" % hp, [128, 2, 129], F32, scope=sp_)
            Cbf = P.sb("Cbf_%d" % hp, [128, 2, 132], BF16, scope=sp_)
            tmpC = P.sb("tmpC_%d" % hp, [128, 2, 129], F32, scope=sp_)
            b_C32 = [P.buf("C32") for _ in range(2)]
            b_Cbf = [P.buf("Cbf") for _ in range(2)]
            b_tmpC = [P.buf("tmpC") for _ in range(2)]
            vt = P.sb("vt_%d" % hp, [128, 2, 2, 132], BF16, scope=sp_)
            b_vt = [P.buf("vt") for _ in range(2)]
            P.op("dve", lambda e: e.memset(C32[:], 0.0), writes=b_C32)
            P.checkpoint(31)
            for d_ in range(2):
                P.op("act", lambda e, d_=d_: e.activation(out=Cbf[:, d_, 0:129], in_=C32[:, d_, :], func=AF.Copy), reads=[b_C32[d_]], writes=[b_Cbf[d_]])
            P.checkpoint(32)
            if pass2:
                wq = P.sb("wq%d" % hp, [128, 8, 128], BF16, scope=sp_)
                wo = P.sb("wo%d" % hp, [128, 8, 256], BF16, scope=sp_)
                b_wq, b_wo = P.buf("wq"), P.buf("wo")
                P.dma("pool", wq[:], w_in_v[:, :, hp * 128:(hp + 1) * 128], writes=[b_wq])
                P.dma("pool", wo[:], w_in_v[:, :, 2048 + hp * 256:2048 + (hp + 1) * 256], writes=[b_wo])
                qT = P.sb("qT%d" % hp, [128, TOK], BF16, scope=sp_)
                kT = P.sb("kT%d" % hp, [128, TOK], BF16, scope=sp_)
                b_qT, b_kT = P.buf("qT"), P.buf("kT")
                sgo = P.sb("sgo%d" % hp, [128, 16, 256], BF16, scope=sp_)
                b_sgo = P.buf("sgo")
                hsum = P.sb("hsum%d" % hp, [128, 16, 2, 128], F32, scope=sp_)
                b_hsum = [P.buf("hsum") for _ in range(16)]
                P.op("pool", lambda e: e.memset(hsum[:], 0.0), writes=b_hsum)
                STm = P.sb("STm%d" % hp, [128, 2, 2, 128], BF16, scope=sp_)
                b_STm = [P.buf("STm") for _ in range(2)]
                STf = P.sb("STf%d" % hp, [128, 2, 2, 128], F32, scope=sp_)
                b_STf = [P.buf("STf") for _ in range(2)]
                small = P.sb("sm%d" % hp, [128, 2, 8], F32, scope=sp_)
                b_small = [P.buf("small") for _ in range(2)]
                Bst = P.sb("Bst%d" % hp, [128, NCORES, 2, 129], F32, scope=sp_)
                b_Bst = P.buf("Bst")
                for i in range(NCORES):
                    P.dma("sp", Bst[:, i], cl_d[i, hp].rearrange("d p e -> p d e"), writes=[b_Bst])
                for i in range(NCORES):
                    for d in range(2):
                        P.op("dve", lambda e, i=i, d=d: e.tensor_scalar(out=Bst[:, i, d, :], in0=Bst[:, i, d, :], scalar1=cm[:, d * 8 + i:d * 8 + i + 1],
                                                                        scalar2=None, op0=ALU.mult), reads=[b_Bst, b_cm], writes=[b_Bst])
                fin = P.sb("fin%d" % hp, [128, 2, 128], F32, scope=sp_)
                finb = P.sb("finb%d" % hp, [128, 2, 128], BF16, scope=sp_)
                b_fin, b_finb = P.buf("fin"), P.buf("finb")
                hnps = nc.psum_tensor
                identb = P.sb("identb%d" % hp, [128, 128], BF16, scope=sp_)
                b_identb = P.buf("identb")
                P.op("dve", lambda e: e.tensor_copy(out=identb[:], in_=C.ident[:]), reads=[C.b_ident], writes=[b_identb])
                for gi in range(4):
                    c0 = gi * 512
                    for (wmat, b_w, dst, b_dst, scl, bi) in ((wq, b_wq, qT, b_qT, 0.125, 4), (wk, b_wk, kT, b_kT, 1.0, 5)):
                        def mm(e, wmat=wmat, c0=c0, bi=bi):
                            ins = None
                            for k in range(8):
                                ins = e.matmul(P.bank[bi][:, 0:512], lhsT=wmat[:, k, :], rhs=hT[:, k, c0:c0 + 512], start=(k == 0), stop=(k == 7))
                            return ins
                        P.op("pe", mm, reads=[b_w, b_hT], writes=[P.b_bank[bi]])
                        P.op("act", lambda e, dst=dst, c0=c0, bi=bi, scl=scl: e.activation(out=dst[:, c0:c0 + 512], in_=P.bank[bi][:, 0:512],
                                                                                         func=AF.Copy, scale=scl), reads=[P.b_bank[bi]], writes=[b_dst])
            tiles = list(range(NT1)) if pass2 else own_tiles
            for t in tiles:
                bi = 6 + (t % 2)

                def mmk(e, t=t, bi=bi):
                    ins = None
                    for k in range(8):
                        ins = e.matmul(P.bank[bi][:, 0:128], lhsT=hT[:, k, t * 128:(t + 1) * 128], rhs=wk[:, k, :], start=(k == 0), stop=(k == 7))
                    return ins

                def mmv(e, t=t, bi=bi):
                    ins = None
                    for k in range(8):
                        ins = e.matmul(P.bank[bi][:, 128:384], lhsT=hT[:, k, t * 128:(t + 1) * 128], rhs=wv[:, k, :], start=(k == 0), stop=(k == 7))
                    return ins
                P.op("pe", mmk, reads=[b_wk, b_hT], writes=[P.b_bank[bi]])
                P.checkpoint(33)
                P.op("pe", mmv, reads=[b_wv, b_hT], writes=[P.b_bank[bi]])
                P.checkpoint(34)
                P.op("act", lambda e, t=t, bi=bi: e.activation(out=k_tm[:, t, :], in_=P.bank[bi][:, 0:128], func=AF.Copy),
                     reads=[P.b_bank[bi]], writes=[b_ktm])
                P.checkpoint(35)
                P.op("act", lambda e, t=t, bi=bi: e.activation(out=v_tm[:, t, :], in_=P.bank[bi][:, 128:384], func=AF.Copy),
                     reads=[P.b_bank[bi]], writes=[b_vtm])
                P.checkpoint(36)
                if t == 3:
                    P.checkpoint(37)
                if pass2 and t < 16:
                    def mmo(e, t=t):
                        ins = None
                        for k in range(8):
                            ins = e.matmul(P.bank[5][:, 0:256], lhsT=hT[:, k, t * 128:(t + 1) * 128], rhs=wo[:, k, :], start=(k == 0), stop=(k == 7))
                        return ins
                    P.op("pe", mmo, reads=[b_wo, b_hT], writes=[P.b_bank[5]])
                    P.op("act", lambda e, t=t: e.activation(out=sgo[:, t, :], in_=P.bank[5][:, 0:256], func=AF.Sigmoid),
                         reads=[P.b_bank[5]], writes=[b_sgo])

            P.checkpoint(4)

            def unit(t, d, full):
                h0 = 2 * hp
                ST_b, H_b, X_b = (2, 3, 4) if d == 0 else (5, 6, 7)
                P.op("dve", lambda e: e.tensor_tensor(out=vt[:, d, :, 0:128], in0=v_tm[:, t, :].rearrange("p (h e) -> p h e", h=2),
                                                      in1=ew[:, t, d, h0:h0 + 2].unsqueeze(2).to_broadcast([128, 2, 128]), op=ALU.mult),
                     reads=[b_vtm, b_ew], writes=[b_vt[d]])
                P.op("dve", lambda e: e.tensor_copy(out=vt[:, d, :, 128], in_=ew[:, t, d, h0:h0 + 2]), reads=[b_ew], writes=[b_vt[d]])
                if full:
                    cs = slice(t * 128, (t + 1) * 128)

                    def mst(e):
                        ins = None
                        for h in range(2):
                            ins = e.matmul(P.bank[STb2[h]][:, 0:128], lhsT=kT[h * 64:(h + 1) * 64, cs], rhs=qT[h * 64:(h + 1) * 64, cs],
                                           start=True, stop=True)
                        return ins
                    STb2 = (ST_b, 0 if d == 0 else 1)
                    P.op("pe", mst, reads=[b_kT, b_qT], writes=[P.b_bank[STb2[0]], P.b_bank[STb2[1]]])
                    P.checkpoint(521)
                    for h in range(2):
                        P.op("act", lambda e, h=h: e.activation(out=STf[:, d, h, :], in_=P.bank[STb2[h]][:, 0:128], func=AF.Copy),
                             reads=[P.b_bank[STb2[h]]], writes=[b_STf[d]])
                    P.op("pool", lambda e: e.tensor_tensor(out=STm[:, d], in0=STf[:, d],
                                                           in1=Lm[d][:].unsqueeze(1).to_broadcast([128, 2, 128]), op=ALU.mult),
                         reads=[b_STf[d], b_L], writes=[b_STm[d]])
                    P.checkpoint(523)

                    def mh(e):
                        ins = None
                        for h in range(2):
                            e.matmul(P.bank[H_b][:, h * 256:h * 256 + 129], lhsT=STm[:, d, h, :], rhs=vt[:, d, h, 0:129], start=True, stop=False)
                            ins = e.matmul(P.bank[H_b][:, h * 256:h * 256 + 129], lhsT=qT[h * 64:(h + 1) * 64, cs],
                                           rhs=Cbf[h * 64:(h + 1) * 64, d, 0:129], start=False, stop=True)
                        return ins
                    P.op("pe", mh, reads=[b_STm[d], b_vt[d], b_qT, b_Cbf[d]], writes=[P.b_bank[H_b]])
                    P.checkpoint(524)
                    Hv = P.bank[H_b][:].rearrange("p (h c) -> p h c", h=2)
                    sm = small[:, d, :]
                    P.op("dve", lambda e: e.tensor_tensor(out=sm[:, 0:2], in0=Hv[:, :, 128], in1=ec[:, t, d, h0:h0 + 2], op=ALU.mult),
                         reads=[P.b_bank[H_b], b_ec], writes=[b_small[d]])
                    P.op("dve", lambda e: e.tensor_scalar(out=sm[:, 2:4], in0=sm[:, 0:2], scalar1=-1.0, scalar2=None, op0=ALU.mult),
                         reads=[b_small[d]], writes=[b_small[d]])
                    P.op("dve", lambda e: e.tensor_tensor(out=sm[:, 2:4], in0=sm[:, 2:4], in1=sm[:, 0:2], op=ALU.max),
                         reads=[b_small[d]], writes=[b_small[d]])
                    P.op("dve", lambda e: e.tensor_scalar(out=sm[:, 2:4], in0=sm[:, 2:4], scalar1=1.0, scalar2=None, op0=ALU.max),
                         reads=[b_small[d]], writes=[b_small[d]])
                    P.op("dve", lambda e: e.reciprocal(out=sm[:, 4:6], in_=sm[:, 2:4]), reads=[b_small[d]], writes=[b_small[d]])
                    P.op("dve", lambda e: e.tensor_tensor(out=sm[:, 6:8], in0=sm[:, 4:6], in1=ec[:, t, d, h0:h0 + 2], op=ALU.mult),
                         reads=[b_small[d], b_ec], writes=[b_small[d]])
                    for h in range(2):
                        P.op("dve", lambda e, h=h: e.scalar_tensor_tensor(out=hsum[:, t, h, :], in0=Hv[:, h, 0:128], scalar=sm[:, 6 + h:7 + h],
                                                                          in1=hsum[:, t, h, :], op0=ALU.mult, op1=ALU.add),
                             reads=[P.b_bank[H_b], b_small[d], b_hsum[t]], writes=[b_hsum[t]])

                def mx(e):
                    ins = None
                    for h in range(2):
                        ins = e.matmul(P.bank[X_b][:, h * 256:h * 256 + 129], lhsT=k_tm[:, t, :], rhs=vt[:, d, h, 0:129], start=True, stop=True)
                    return ins
                P.op("pe", mx, reads=[b_ktm, b_vt[d]], writes=[P.b_bank[X_b]])
                Xv = P.bank[X_b][:].rearrange("p (h c) -> p h c", h=2)
                for h in range(2):
                    ps_ = slice(h * 64, (h + 1) * 64)
                    P.op("dve", lambda e, h=h, ps_=ps_: e.tensor_tensor(out=tmpC[ps_, d, :], in0=Xv[ps_, h, 0:129], in1=C32[ps_, d, :], op=ALU.add),
                         reads=[P.b_bank[X_b], b_C32[d]], writes=[b_tmpC[d]])
                P.op("dve", lambda e: e.tensor_scalar(out=C32[:, d, :], in0=tmpC[:, d, :], scalar1=dec[:, hp * 2 + d, t:t + 1], scalar2=None, op0=ALU.mult),
                     reads=[b_tmpC[d], b_dec], writes=[b_C32[d]])
                P.op("act", lambda e: e.activation(out=Cbf[:, d, 0:129], in_=C32[:, d, :], func=AF.Copy), reads=[b_C32[d]], writes=[b_Cbf[d]])

            if pass2:
                for t in (16, 17):
                    unit(t, 0, False)
                for t in (17, 16):
                    unit(t, 1, False)
                for d in range(2):
                    order = range(NCORES) if d == 0 else range(NCORES - 1, -1, -1)
                    for i in order:
                        P.op("dve", lambda e, i=i, d=d: e.scalar_tensor_tensor(
                            out=C32[:, d, :], in0=C32[:, d, :], scalar=Dst[:, i, hp * 2 + d:hp * 2 + d + 1], in1=Bst[:, i, d, :],
                            op0=ALU.mult, op1=ALU.add), reads=[b_C32[d], b_Dst, b_Bst], writes=[b_C32[d]])
                    P.op("act", lambda e, d=d: e.activation(out=Cbf[:, d, 0:129], in_=C32[:, d, :], func=AF.Copy), reads=[b_C32[d]], writes=[b_Cbf[d]])
            P.checkpoint(51)
            for i in range(16):
                unit(i, 0, pass2)
                if i == 0:
                    P.checkpoint(52)
                unit(15 - i, 1, pass2)
                if i == 0:
                    P.checkpoint(5)
            P.checkpoint(6)
            if not pass2:
                P.out_toks.append(P.dma("sp", clo_d[hp].rearrange("d p e -> p d e"), C32[:], reads=b_C32, writes=[b_out]))
            else:
                hb = P.bank[1][:].bitcast(BF16)
                for t in range(16):
                    P.op("pool", lambda e, t=t: e.tensor_tensor(out=fin[:], in0=hsum[:, t], in1=hsum[:, t], op=ALU.mult),
                         reads=[b_hsum[t]], writes=[b_fin])
                    P.op("dve", lambda e: e.tensor_reduce(out=small[:, 0, 0:2], in_=fin[:], axis=AX.X, op=ALU.add), reads=[b_fin], writes=[b_small[0]])
                    P.op("act", lambda e: e.activation(out=small[:, 0, 2:4], in_=small[:, 0, 0:2], func=AF.Ln, scale=1.0 / 128, bias=C.epsc[:, 0:1]),
                         reads=[b_small[0], C.b_eps], writes=[b_small[0]])
                    P.op("act", lambda e: e.activation(out=small[:, 0, 2:4], in_=small[:, 0, 2:4], func=AF.Exp, scale=-0.5),
                         reads=[b_small[0]], writes=[b_small[0]])
                    P.op("dve", lambda e, t=t: e.tensor_tensor(out=fin[:], in0=hsum[:, t], in1=small[:, 0, 2:4].unsqueeze(2).to_broadcast([128, 2, 128]),
                                                               op=ALU.mult), reads=[b_hsum[t], b_small[0]], writes=[b_fin])
                    P.op("dve", lambda e: e.tensor_tensor(out=fin[:], in0=fin[:], in1=ngbc[:, hp * 256:(hp + 1) * 256].rearrange("p (h e) -> p h e", h=2),
                                                          op=ALU.mult), reads=[b_fin, b_ngbc], writes=[b_fin])
                    P.op("dve", lambda e, t=t: e.tensor_tensor(out=finb[:], in0=fin[:], in1=sgo[:, t, :].rearrange("p (h e) -> p h e", h=2),
                                                               op=ALU.mult), reads=[b_fin, b_sgo], writes=[b_finb])

                    def trh(e):
                        ins = None
                        for h in range(2):
                            ins = e.transpose(hb[:, h * 128:(h + 1) * 128], finb[:, h, :], identb[:, :])
                        return ins
                    P.op("pe", trh, reads=[b_finb, b_identb], writes=[P.b_bank[1]])
                    P.op("act", lambda e, t=t: e.activation(out=hnT[:, 2 * hp:2 * hp + 2, t * 128:(t + 1) * 128],
                                                            in_=hb[:, 0:256].rearrange("p (h t) -> p h t", h=2), func=AF.Copy),
                         reads=[P.b_bank[1]], writes=[b_hnT])
                    P.checkpoint(61)
            P.barrier()
    if not pass2:
        P.op("dve", lambda e: e.tensor_reduce(out=dtc[:], in_=csel[:, :, 0:16], axis=AX.X, op=ALU.add), reads=[b_csel], writes=[b_dtc])
        P.op("act", lambda e: e.activation(out=dtc[:], in_=dtc[:], func=AF.Exp), reads=[b_dtc], writes=[b_dtc])
        P.out_toks.append(P.dma("sp", dto_d, dtc[:], reads=[b_dtc], writes=[b_out]))
        P.finish()
        sc_hT.close()
        P.es.close()
        return nc
    sc_hT.close()

    x1s = nc.dram_tensor("x1s", [128, 8, TOK], F32).ap()
    b_x1s = P.buf("x1s")
    sc_o = ExitStack()
    wout = P.sb("wout", [128, 8, D], BF16, scope=sc_o)
    b_wout = P.buf("wout")
    P.dma("pool", wout[:], w_out_d.rearrange("(c p) n -> p c n", p=128), writes=[b_wout])
    xo = P.sb("xo", [128, 8, 512], F32, scope=sc_o)
    b_xo = P.buf("xo")
    yi = 0
    for gi in range(4):
        c0 = gi * 512
        load_tokens_T(P, C, x_d[c0:c0 + 512, :], 512, xo, b_xo, 0, xt_tiles, b_xt, tps, b_tps, tcount)
        for oc in range(8):
            bi = 4 + (yi % 2)
            yi += 1

            def mm(e, bi=bi, oc=oc, c0=c0):
                ins = None
                for jj in range(8):
                    ins = e.matmul(P.bank[bi][:, 0:512], lhsT=wout[:, jj, oc * 128:(oc + 1) * 128], rhs=hnT[:, jj, c0:c0 + 512],
                                   start=(jj == 0), stop=(jj == 7))
                return ins
            P.op("pe", mm, reads=[b_wout, b_hnT], writes=[P.b_bank[bi]])
            P.op("dve", lambda e, bi=bi, oc=oc: e.scalar_tensor_tensor(
                out=xo[:, oc, :], in0=P.bank[bi][:, 0:512], scalar=mod[:, 0, 2, oc:oc + 1], in1=xo[:, oc, :],
                op0=ALU.mult, op1=ALU.add), reads=[P.b_bank[bi], b_xo, b_mod], writes=[b_xo])
        P.dma("sp", x1s[:, :, c0:c0 + 512], xo[:, :, :], reads=[b_xo], writes=[b_x1s])
    P.barrier()
    sc_o.close()
    sc_hn.close()
    sc2 = ExitStack()
    xT = P.sb("xT", [128, 8, TOK], F32, scope=sc2)
    b_xT = P.buf("xT")
    for k in range(8):
        P.dma("sp", xT[:, k, :], x1s[:, k, :], reads=[b_x1s], writes=[b_xT])
    with ExitStack() as sc3:
        def gmods(r):
            return (lambda k: gm[:, r, 1, k:k + 1], lambda k: mod[:, r, 3, k:k + 1], lambda k: mod[:, r, 5, k:k + 1], [b_gm, b_mod])
        mgroups = [(g * 512, 512, 0) for g in range(4)]
        moe(P, C, sc3, xT, b_xT, mgroups, gmods, wr_d, br_d, sel_d, wg_d, wu_d, wd_d, TOK)
        P.barrier()
    with ExitStack() as sc4:
        sq = P.sb("f_sq", [128, 8, 512], F32, scope=sc4)
        b_sq = P.buf("f_sq")
        rstd = P.sb("f_rstd", [128, 512], F32, scope=sc4)
        b_rstd = P.buf("f_rstd")
        for gi in range(4):
            c0 = gi * 512
            norm_fm(P, C, xT, b_xT, c0, 512, sq, b_sq, P.bank[0], P.b_bank[0], rstd, b_rstd,
                    lambda k: sv[:, 16 + k:17 + k], lambda k: sv[:, 24 + k:25 + k], [b_sv], [(xT, b_xT, c0)])
        store_tokens(P, C, xT, b_xT, 0, TOK, out_d, tps, b_tps, xt_tiles, b_xt, b_out, tcount)
        P.finish()
    sc2.close()
    P.es.close()
    return nc


def _consts():
    ident = np.eye(128, dtype=np.float32)
    sel = np.zeros((32, 32, 128), np.float32)
    for e in range(32):
        sel[e, e, :] = 1.0
    return ident, sel


def run_l0(inputs, pass2, st_all=None):
    f = lambda k: np.ascontiguousarray(np.asarray(inputs[k], dtype=np.float32))
    x = f("x")[0]
    ctx = f("ctx")[0]
    ident, sel = _consts()
    cc = np.stack([f("c")[0], f("c_ctx")], 0)
    nc = build_l0(pass2)
    in_maps = []
    for k in range(NCORES):
        s = k * TOK
        xh = np.zeros((3, D), np.float32)
        hm = np.zeros((128, 3), np.float32)
        if k > 0:
            xh[0:2] = x[s - 2:s]
            hm[:, 0:2] = 1.0
        if k < NCORES - 1:
            xh[2] = x[s + TOK]
            hm[:, 2] = 1.0
        m = {"x": x[s:s + TOK], "xh": xh, "hmask": hm, "ctx": ctx, "cc": cc, "ident": ident,
             "ada_w": f("l0_ada_w"), "ada_b": f("l0_ada_b"), "g1": f("l0_norm1_g"), "w_in": f("l0_rg_w_in"),
             "conv_w": f("l0_rg_conv_w"), "conv_b": f("l0_rg_conv_b"), "w_a": f("l0_rg_w_a"), "b_a": f("l0_rg_b_a"),
             "w_x": f("l0_rg_w_x"), "b_x": f("l0_rg_b_x"), "lam": f("l0_rg_lambda")}
        if pass2:
            cm = np.zeros((128, 16), np.float32)
            cm[:, 0:k] = 1.0
            cm[:, 8 + k + 1:16] = 1.0
            m.update({"g2": f("l0_norm2_g"), "w_out": f("l0_rg_w_out"), "st_all": st_all, "cmask": cm,
                      "wr": np.ascontiguousarray(np.concatenate([f("l0_moe_w_grp"), f("l0_moe_w_exp")], 1)),
                      "br": np.ascontiguousarray(np.concatenate([f("l0_moe_b_grp"), f("l0_moe_b_exp")])[None, :]),
                      "sel": sel, "wg": f("l0_moe_w_gate"), "wu": f("l0_moe_w_up"), "wd": f("l0_moe_w_down")})
        in_maps.append(m)
    res = run_bass_kernel_spmd(nc, in_maps, core_ids=list(range(NCORES)))
    return res.results


def _consts1():
    r = np.arange(128)
    Lf = (r[:, None] <= r[None, :]).astype(np.float32)
    Lb = (r[:, None] >= r[None, :]).astype(np.float32)
    esel = np.zeros((32, 8, 128), np.float32)
    for hp in range(4):
        for d in range(2):
            for m in range(128):
                esel[(2 * d + 1) * 8 + 2 * hp + (m // 64), hp * 2 + d, m] = 1.0
    return Lf, Lb, esel


def to_colmajor(t):
    n, d = t.shape
    return np.ascontiguousarray(t.reshape(n // 64, 64, d).transpose(1, 0, 2).reshape(n, d))


def from_colmajor(t):
    n, d = t.shape
    return np.ascontiguousarray(t.reshape(64, n // 64, d).transpose(1, 0, 2).reshape(n, d))


def run_l1(inputs, xs, ctx2, pass2, cl_all=None, dt_all=None):
    f = lambda k: np.ascontiguousarray(np.asarray(inputs[k], dtype=np.float32))
    ident, sel = _consts()
    Lf, Lb, esel = _consts1()
    cc = np.stack([f("c")[0], f("c_ctx")], 0)
    nc = build_l1(pass2)
    in_maps = []
    for k in range(NCORES):
        s = k * TOK
        m = {"x": np.ascontiguousarray(xs[s:s + TOK]), "ctx": ctx2, "cc": cc, "ident": ident,
             "ada_w": f("l1_ada_w"), "ada_b": f("l1_ada_b"), "g1": f("l1_norm1_g"), "w_in": f("l1_ml_w_in"),
             "bg": f("l1_ml_b_gates")[None, :], "Lf": Lf, "Lb": Lb, "esel": esel}
        if pass2:
            cm = np.zeros((128, 16), np.float32)
            cm[:, 0:k] = 1.0
            cm[:, 8 + k + 1:16] = 1.0
            m.update({"g2": f("l1_norm2_g"), "gf": f("final_norm_g"), "ng": f("l1_ml_norm_g")[None, :], "w_out": f("l1_ml_w_out"),
                      "cl_all": cl_all, "dt_all": dt_all, "cmask": cm,
                      "wr": np.ascontiguousarray(np.concatenate([f("l1_moe_w_grp"), f("l1_moe_w_exp")], 1)),
                      "br": np.ascontiguousarray(np.concatenate([f("l1_moe_b_grp"), f("l1_moe_b_exp")])[None, :]),
                      "sel": sel, "wg": f("l1_moe_w_gate"), "wu": f("l1_moe_w_up"), "wd": f("l1_moe_w_down")})
        in_maps.append(m)
    res = run_bass_kernel_spmd(nc, in_maps, core_ids=list(range(NCORES)))
    return res.results


def kernel(**inputs):
    r1 = run_l0(inputs, False)
    st_all = np.ascontiguousarray(np.concatenate([r["st_out"] for r in r1], 0))
    r2 = run_l0(inputs, True, st_all)
    x2 = np.concatenate([r["x2"] for r in r2], 0)
    ctx2 = np.ascontiguousarray(r2[0]["ctx2"])
    xs = to_colmajor(x2)
    r3 = run_l1(inputs, xs, ctx2, False)
    cl_all = np.ascontiguousarray(np.stack([r["cl_out"] for r in r3], 0))
    dt_all = np.ascontiguousarray(np.stack([r["dt_out"] for r in r3], 0))
    r4 = run_l1(inputs, xs, ctx2, True, cl_all, dt_all)
    out = from_colmajor(np.concatenate([r["out"] for r in r4], 0))
    return out[None].astype(np.float32)
```
